# Optimizing a Trainium2 kernel written in Bass

```python
import math
import jax, jax.numpy as jnp
from jax import lax
import numpy as np


D_MODEL = 1024
BATCH = 8
SEQ = 4096
DEPTH = 1

CHUNK = 64
N_META = 16
Q_BLOCK = 128
MIX_WIDTH = D_MODEL
HG_WIDTH = MIX_WIDTH // 2
HG_HEADS = 4
HG_DK = HG_WIDTH // HG_HEADS
SB_WIDTH = MIX_WIDTH - HG_WIDTH
SB_HEADS = 8
SB_DH = SB_WIDTH // SB_HEADS
N_GROUPS = 4
EXPERTS_PER_GROUP = 4
N_EXPERTS = N_GROUPS * EXPERTS_PER_GROUP
TOP_K_INNER = 2
D_EXPERT = D_MODEL // 2
ALPHA = (2 * DEPTH) ** 0.25
BETA = (8 * DEPTH) ** -0.25
LN_EPS = 1e-5
RMS_EPS = 1e-6
SPLITS = (HG_WIDTH, 2 * HG_WIDTH, 3 * HG_WIDTH, 4 * HG_WIDTH,
          4 * HG_WIDTH + SB_WIDTH, 4 * HG_WIDTH + 2 * SB_WIDTH)
IN_COLS = 4 * HG_WIDTH + 3 * SB_WIDTH

kernel_name = 'hymba_hgrn2_stickbreak_hmoe_deepnorm'


def layer_norm(x, g, b):
    xf = x.astype(jnp.float32)
    mu = jnp.mean(xf, axis=-1, keepdims=True)
    var = jnp.mean(jnp.square(xf - mu), axis=-1, keepdims=True)
    return ((xf - mu) * lax.rsqrt(var + LN_EPS) * g + b).astype(x.dtype)


def head_rms_norm(x, g):
    xf = x.astype(jnp.float32)
    y = xf * lax.rsqrt(jnp.mean(xf * xf, axis=-1, keepdims=True) + RMS_EPS)
    return y.reshape(*x.shape[:-2], -1) * g


def hgrn2_mixer(q_raw, f_raw, i_raw, g_raw, lb, norm_g):
    B, Lp, _ = q_raw.shape
    nc = Lp // CHUNK
    zf = f_raw.astype(jnp.float32)
    log_f = jnp.log(lb + (1.0 - lb) * jax.nn.sigmoid(zf))
    k = (1.0 - lb) * jax.nn.sigmoid(-zf)
    q = jax.nn.silu(q_raw.astype(jnp.float32))
    v = i_raw.astype(jnp.float32)

    def to_chunks(a):
        return a.reshape(B, nc, CHUNK, HG_HEADS, HG_DK).transpose(1, 0, 3, 2, 4)

    qc, kc, vc, lfc = to_chunks(q), to_chunks(k), to_chunks(v), to_chunks(log_f)
    causal = jnp.tril(jnp.ones((CHUNK, CHUNK), dtype=bool))

    def step(S, inp):
        qb, kb, vb, lfb = inp
        bcum = jnp.cumsum(lfb, axis=2)
        diff = bcum[:, :, :, None, :] - bcum[:, :, None, :, :]
        decay = jnp.exp(jnp.where(causal[:, :, None], diff, -jnp.inf))
        scores = jnp.einsum('bhtd,bhsd,bhtsd->bhts', qb, kb, decay)
        o = (jnp.einsum('bhts,bhsv->bhtv', scores, vb)
             + jnp.einsum('bhtd,bhdv->bhtv', qb * jnp.exp(bcum), S))
        b_last = bcum[:, :, -1:, :]
        S_new = (jnp.exp(b_last[:, :, 0, :, None]) * S
                 + jnp.einsum('bhsd,bhsv->bhdv', kb * jnp.exp(b_last - bcum), vb))
        return S_new, o

    S0 = jnp.zeros((B, HG_HEADS, HG_DK, HG_DK), jnp.float32)
    _, oc = lax.scan(step, S0, (qc, kc, vc, lfc))
    o = oc.transpose(1, 0, 3, 2, 4).reshape(B, Lp, HG_HEADS, HG_DK)
    gate = jax.nn.sigmoid(g_raw.astype(jnp.float32))
    return head_rms_norm(o, norm_g) * gate


def stick_breaking_mixer(q, k, v, norm_g):
    B, Lp, _ = q.shape

    def heads(a):
        return a.astype(jnp.float32).reshape(B, Lp, SB_HEADS, SB_DH).transpose(0, 2, 1, 3)

    qh = heads(q) * (SB_DH ** -0.5)
    kh, vh = heads(k), heads(v)
    outs = []
    for blk in range(Lp // Q_BLOCK):
        start = blk * Q_BLOCK
        end = start + Q_BLOCK
        z = jnp.einsum('bhqd,bhkd->bhqk', qh[:, :, start:end], kh[:, :, :end])
        mask = jnp.arange(end)[None, :] < (start + jnp.arange(Q_BLOCK))[:, None]
        log_keep = jnp.where(mask, jax.nn.log_sigmoid(-z), 0.0)
        log_a = jax.nn.log_sigmoid(z) + lax.cumsum(log_keep, axis=3, reverse=True) - log_keep
        a = jnp.where(mask, jnp.exp(log_a), 0.0)
        outs.append(jnp.einsum('bhqk,bhkd->bhqd', a, vh[:, :, :end]))
    o = jnp.concatenate(outs, axis=2).transpose(0, 2, 1, 3)
    return head_rms_norm(o, norm_g)


def hierarchical_moe(h, w_rg, b_rg, w_re, b_re, w1, w3, w2):
    B, L, D = h.shape
    xf = h.reshape(-1, D)
    logits_g = (xf @ w_rg).astype(jnp.float32) + b_rg
    probs_g = jax.nn.softmax(logits_g, axis=-1)
    _, grp = lax.top_k(logits_g, 1)
    grp_oh = jax.nn.one_hot(grp[:, 0], N_GROUPS, dtype=jnp.float32)
    p_grp = jnp.sum(probs_g * grp_oh, axis=-1, keepdims=True)
    logits_e = jnp.einsum('nd,dge->nge', xf, w_re).astype(jnp.float32) + b_re
    logits_in = jnp.einsum('nge,ng->ne', logits_e, grp_oh)
    top_v, top_i = lax.top_k(logits_in, TOP_K_INNER)
    w_inner = jax.nn.softmax(top_v, axis=-1)
    expert_id = grp * EXPERTS_PER_GROUP + top_i
    gates = p_grp * jnp.sum(jax.nn.one_hot(expert_id, N_EXPERTS, dtype=jnp.float32)
                            * w_inner[..., None], axis=1)
    y = jnp.zeros(xf.shape, jnp.float32)
    for e in range(N_EXPERTS):
        hid = jax.nn.silu(xf @ w1[e]) * (xf @ w3[e])
        y = y + gates[:, e:e + 1] * (hid @ w2[e])
    return y.astype(h.dtype).reshape(B, L, D)


def setup_inputs(seed: int = 0) -> dict:
    key = jax.random.key(seed)
    ks = jax.random.split(key, 18)
    f32 = jnp.float32
    col_scale = jnp.concatenate([
        jnp.ones((2 * HG_WIDTH,), f32), jnp.full((HG_WIDTH,), BETA, f32),
        jnp.ones((HG_WIDTH + 2 * SB_WIDTH,), f32),
        jnp.full((SB_WIDTH,), BETA, f32)])
    w_in = jax.random.normal(ks[2], (DEPTH, D_MODEL, IN_COLS), f32) * (D_MODEL ** -0.5) * col_scale
    return {
        'x': jax.random.normal(ks[0], (BATCH, SEQ, D_MODEL), f32),
        'meta_tokens': jax.random.normal(ks[1], (N_META, D_MODEL), f32),
        'w_in': w_in,
        'hg_lower_bound': 0.1 * jax.random.normal(ks[3], (DEPTH + 1, HG_WIDTH), f32),
        'hg_norm_g': 1.0 + 0.02 * jax.random.normal(ks[4], (DEPTH, HG_WIDTH), f32),
        'sb_norm_g': 1.0 + 0.02 * jax.random.normal(ks[5], (DEPTH, SB_WIDTH), f32),
        'w_out': jax.random.normal(ks[6], (DEPTH, MIX_WIDTH, D_MODEL), f32) * (MIX_WIDTH ** -0.5) * BETA,
        'ln1_g': 1.0 + 0.02 * jax.random.normal(ks[7], (DEPTH, D_MODEL), f32),
        'ln1_b': 0.02 * jax.random.normal(ks[8], (DEPTH, D_MODEL), f32),
        'w_router_group': jax.random.normal(ks[9], (DEPTH, D_MODEL, N_GROUPS), f32) * (D_MODEL ** -0.5),
        'b_router_group': 0.01 * jax.random.normal(ks[10], (DEPTH, N_GROUPS), f32),
        'w_router_expert': jax.random.normal(ks[11], (DEPTH, D_MODEL, N_GROUPS, EXPERTS_PER_GROUP), f32) * (D_MODEL ** -0.5),
        'b_router_expert': 0.01 * jax.random.normal(ks[12], (DEPTH, N_GROUPS, EXPERTS_PER_GROUP), f32),
        'w_exp_gate': jax.random.normal(ks[13], (DEPTH, N_EXPERTS, D_MODEL, D_EXPERT), f32) * (D_MODEL ** -0.5) * BETA,
        'w_exp_up': jax.random.normal(ks[14], (DEPTH, N_EXPERTS, D_MODEL, D_EXPERT), f32) * (D_MODEL ** -0.5) * BETA,
        'w_exp_down': jax.random.normal(ks[15], (DEPTH, N_EXPERTS, D_EXPERT, D_MODEL), f32) * (D_EXPERT ** -0.5) * BETA,
        'ln2_g': 1.0 + 0.02 * jax.random.normal(ks[16], (DEPTH, D_MODEL), f32),
        'ln2_b': 0.02 * jax.random.normal(ks[17], (DEPTH, D_MODEL), f32),
    }


def reference(x, meta_tokens, w_in, hg_lower_bound, hg_norm_g, sb_norm_g, w_out, ln1_g, ln1_b,
              w_router_group, b_router_group, w_router_expert, b_router_expert,
              w_exp_gate, w_exp_up, w_exp_down, ln2_g, ln2_b):
    B = x.shape[0]
    meta = jnp.broadcast_to(meta_tokens[None].astype(x.dtype), (B, N_META, D_MODEL))
    h = jnp.concatenate([meta, x], axis=1)
    L = h.shape[1]
    Lp = -(-L // Q_BLOCK) * Q_BLOCK
    lb_all = jnp.cumsum(jax.nn.softmax(hg_lower_bound.astype(jnp.float32), axis=0), axis=0)
    for layer in range(DEPTH):
        proj = jnp.einsum('bld,dp->blp', h, w_in[layer])
        proj = jnp.pad(proj, ((0, 0), (0, Lp - L), (0, 0)))
        hq, hf, hi, hg, sq, sk, sv = jnp.split(proj, SPLITS, axis=-1)
        o_hg = hgrn2_mixer(hq, hf, hi, hg, lb_all[layer], hg_norm_g[layer])
        o_sb = stick_breaking_mixer(sq, sk, sv, sb_norm_g[layer])
        mixed = jnp.concatenate([o_hg, o_sb], axis=-1)[:, :L].astype(h.dtype)
        h = layer_norm(ALPHA * h + mixed @ w_out[layer], ln1_g[layer], ln1_b[layer])
        ffn = hierarchical_moe(h, w_router_group[layer], b_router_group[layer],
                               w_router_expert[layer], b_router_expert[layer],
                               w_exp_gate[layer], w_exp_up[layer], w_exp_down[layer])
        h = layer_norm(ALPHA * h + ffn, ln2_g[layer], ln2_b[layer])
    return h[:, N_META:]
```

```python
import os
import numpy as np
import concourse.bass as bass
import concourse.mybir as mybir
from concourse.bass_utils import run_bass_kernel_spmd

F32 = mybir.dt.float32
BF16 = mybir.dt.bfloat16
AF = mybir.ActivationFunctionType
ALU = mybir.AluOpType
AX = mybir.AxisListType

D = 1024
SEQ = 4096
NMETA = 16
L = SEQ + NMETA
NT = 33
LP = NT * 128
SBT = 3
NSB = NT // SBT
W = SBT * 128
NE = 16
ALPHA = 2 ** 0.25
LN_EPS = 1e-5
RMS_EPS = 1e-6
BIG = 1.0e4
SAME_ENGINE_SKIP = 10 ** 9

ENGS = ("pe", "act", "dve", "pool", "sp")


class Buf:
    __slots__ = ("name", "writers", "readers", "dsem", "dcount")

    def __init__(self, name):
        self.name = name
        self.writers = {}
        self.readers = {}
        self.dsem = None
        self.dcount = 0


class Prog:
    def __init__(self, nc):
        self.nc = nc
        self.ops = {e: [] for e in ENGS}
        self.nsem = 0
        self.dma_bufs = []
        self._dsem_of = {}
        self._keep = []
        self.defer = None

    def play(self, item):
        kind, eng, fn, reads, writes = item
        if kind == "op":
            self.op(eng, fn, list(reads), list(writes))
        else:
            self.dma(eng, fn, list(reads), list(writes))

    def _new_dma_sem(self):
        cm = self.nc.semaphore("dq%d" % self.nsem)
        self.nsem += 1
        h = cm.__enter__()
        self._keep.append(cm)
        return h

    def _collect(self, eng, reads, writes):
        deps = {}

        here = len(self.ops[eng])

        def add(k, v):
            if k == eng and eng == "pe":
                return
            if k == eng and eng in ("dve", "act") and here - v >= SAME_ENGINE_SKIP:
                return
            if deps.get(k, -1) < v:
                deps[k] = v
        for b in reads:
            for k, v in b.writers.items():
                add(k, v)
        for b in writes:
            for k, v in b.readers.items():
                add(k, v)
            for k, v in b.writers.items():
                add(k, v)
        return deps

    def _update(self, key, val, reads, writes):
        for b in reads:
            if b.readers.get(key, -1) < val:
                b.readers[key] = val
        for b in writes:
            if b.readers:
                b.readers = {}
                b.writers = {}
            if b.writers.get(key, -1) < val:
                b.writers[key] = val

    def op(self, eng, fn, reads=(), writes=()):
        if self.defer is not None:
            self.defer.append(("op", eng, fn, tuple(reads), tuple(writes)))
            return
        deps = self._collect(eng, reads, writes)
        pos = len(self.ops[eng])
        self.ops[eng].append({"deps": deps, "fn": fn, "dma": None, "need_inc": False})
        self._update(eng, pos, reads, writes)

    def dma(self, eng, fn, reads=(), writes=(), n=1):
        if self.defer is not None:
            self.defer.append(("dma", eng, fn, tuple(reads), tuple(writes)))
            return
        deps = self._collect(eng, reads, writes)
        owner = writes[0] if writes else reads[0]
        cls = "sw" if eng == "pool" else "hw"
        if owner.dsem is None:
            owner.dsem = {}
        if cls not in owner.dsem:
            owner.dsem[cls] = [self._new_dma_sem(), 0]
            self.dma_bufs.append(owner.dsem[cls])
        ent = owner.dsem[cls]
        ent[1] += 16 * n
        key = ("dma", id(owner), cls)
        self._dsem_of[key] = ent[0]
        self.ops[eng].append({"deps": deps, "fn": fn, "dma": ent[0], "need_inc": False})
        self._update(key, ent[1], reads, writes)

    def barrier(self):
        last = {}
        for e in ENGS:
            idx = [i for i, o in enumerate(self.ops[e]) if o["fn"] is not None and o["dma"] is None]
            if idx:
                last[e] = idx[-1]
        for e in ENGS:
            deps = {k: v for k, v in last.items() if k != e}
            self.ops[e].append({"deps": deps, "fn": None, "dma": None, "need_inc": False})

    def emit(self):
        nc = self.nc
        for e in ENGS:
            for o in self.ops[e]:
                for k, v in o["deps"].items():
                    if isinstance(k, str):
                        self.ops[k][v]["need_inc"] = True
        cnt_at = {}
        for e in ENGS:
            c = 0
            arr = []
            for o in self.ops[e]:
                if o["need_inc"]:
                    assert o["fn"] is not None and o["dma"] is None
                    c += 1
                arr.append(c)
            cnt_at[e] = arr
        sems = {}
        for e in ENGS:
            if e == "sp":
                continue
            cm = nc.semaphore("c_" + e)
            sems[e] = cm.__enter__()
            self._keep.append(cm)
        handles = {"pe": "tensor", "act": "scalar", "dve": "vector", "pool": "gpsimd", "sp": "sync"}
        with nc.Block() as block:
            for e in ENGS:
                ops = self.ops[e]

                def body(eh, e=e, ops=ops):
                    seen = {}
                    for o in ops:
                        for k, v in o["deps"].items():
                            if isinstance(k, str):
                                sem = sems[k]
                                val = cnt_at[k][v]
                            else:
                                sem = self._dsem_of[k]
                                val = v
                            if seen.get(sem.num, -1) >= val:
                                continue
                            seen[sem.num] = val
                            eh.wait_ge(sem, val)
                        if o["fn"] is None:
                            continue
                        ins = o["fn"](eh)
                        if o["dma"] is not None:
                            ins.then_inc(o["dma"], 16)
                        elif o["need_inc"]:
                            ins.then_inc(sems[e], 1)
                    if e == "sp":
                        for ent in self.dma_bufs:
                            eh.wait_ge(ent[0], ent[1])
                getattr(block, handles[e])(body)


def host_consts():
    s = np.arange(128)
    same = (s[:, None] // 64) == (s[None, :] // 64)
    c = {}
    c["c_ident"] = np.eye(128, dtype=np.float32)
    c["c_btri"] = (same & (s[:, None] <= s[None, :])).astype(np.float32)
    c["c_negtri"] = -(s[:, None] >= s[None, :]).astype(np.float32)
    c["c_stri"] = (s[:, None] < s[None, :]).astype(np.float32)
    c["c_bones"] = same.astype(np.float32)
    c["c_ones"] = np.ones((128, 128), np.float32)
    ci = np.zeros((128, 2), np.float32)
    ci[:64, 0] = 1.0
    ci[64:, 1] = 1.0
    c["c_cind"] = ci
    return c


def build(nc, dbg=False, maxq=NSB, stop=None, nexp=NE):
    P = Prog(nc)

    def dram_in(name, shape):
        return nc.dram_tensor(name, list(shape), F32, kind="ExternalInput").ap()

    x_d = dram_in("x", [SEQ, D])
    meta_d = dram_in("meta_tokens", [NMETA, D])
    w_in_d = dram_in("w_in", [D, 3584])
    lbp_d = dram_in("hg_lower_bound", [2, 512])
    hgg_d = dram_in("hg_norm_g", [1, 512])
    sbg_d = dram_in("sb_norm_g", [1, 512])
    w_out_d = dram_in("w_out", [D, D])
    ln1g_d = dram_in("ln1_g", [1, D])
    ln1b_d = dram_in("ln1_b", [1, D])
    wrg_d = dram_in("w_router_group", [D, 4])
    brg_d = dram_in("b_router_group", [1, 4])
    wre_d = dram_in("w_router_expert", [D, 16])
    bre_d = dram_in("b_router_expert", [1, 16])
    w1_d = dram_in("w_exp_gate", [NE, D, 512])
    w3_d = dram_in("w_exp_up", [NE, D, 512])
    w2_d = dram_in("w_exp_down", [NE, 512, D])
    ln2g_d = dram_in("ln2_g", [1, D])
    ln2b_d = dram_in("ln2_b", [1, D])
    cst = {k: dram_in(k, v.shape) for k, v in host_consts().items()}
    out_d = nc.dram_tensor("out", [SEQ, D], F32, kind="ExternalOutput").ap()
    if dbg:
        dbg_mixT = nc.dram_tensor("dbg_mixT", [8, 128, LP], BF16, kind="ExternalOutput").ap()
        dbg_h1 = nc.dram_tensor("dbg_h1", [LP, D], F32, kind="ExternalOutput").ap()

    w_in_bf = nc.dram_tensor("w_in_bf", [D, 3584], BF16).ap()
    w_out_bf = nc.dram_tensor("w_out_bf", [D, D], BF16).ap()
    w1_bf = nc.dram_tensor("w1_bf", [NE, D, 512], BF16).ap()
    w3_bf = nc.dram_tensor("w3_bf", [NE, D, 512], BF16).ap()
    w2_bf = nc.dram_tensor("w2_bf", [NE, 512, D], BF16).ap()

    sb_lo = (nc.sbuf_base + 63) // 64 * 64
    sb_hi = nc.sbuf_top
    cur = [sb_lo]
    cnt = [0]

    def sb(shape, dt):
        n = 1
        for s_ in shape[1:]:
            n *= s_
        nbytes = n * (4 if dt == F32 else 2)
        nbytes = (nbytes + 63) // 64 * 64
        off = cur[0]
        cur[0] += nbytes
        assert cur[0] <= sb_hi, ("SBUF overflow", cur[0], sb_hi)
        cnt[0] += 1
        return nc.alloc_sbuf_tensor_at("t%d" % cnt[0], list(shape), dt, offset=off)

    psf = [nc.alloc_psum_tensor("psf%d" % i, [128, 512], F32) for i in range(8)]
    nc.psum_base = 0
    psb = [nc.alloc_psum_tensor("psb%d" % i, [128, 1024], BF16) for i in range(8)]
    PB = [Buf("bank%d" % i) for i in range(8)]

    kT = sb([128, 4, LP], BF16)
    vv = sb([128, NT, 512], BF16)
    BkT = [Buf("kT%d" % i) for i in range(NSB)]
    Bv = [Buf("v%d" % i) for i in range(NSB)]
    ident_b = sb([128, 128], BF16)
    ident_f = sb([128, 128], F32)
    btri_f = sb([128, 128], F32)
    btri_b = sb([128, 128], BF16)
    negtri_b = sb([128, 128], BF16)
    stri_b = sb([128, 128], BF16)
    bones_b = sb([128, 128], BF16)
    ones_b = sb([128, 128], BF16)
    ones_f = sb([128, 128], F32)
    cind_f = sb([128, 2], F32)
    ln1g = sb([128, D], F32)
    ln1b = sb([128, D], F32)
    ln2g = sb([128, D], F32)
    ln2b = sb([128, D], F32)
    lb_rep = sb([128, 512], F32)
    oml_rep = sb([128, 512], F32)
    hgg_rep = sb([128, 512], F32)
    sbgT = sb([128, 4], F32)
    w_r = sb([128, 8, 20], F32)
    b_r = sb([1, 20], F32)
    S_f = sb([128, 4, 128], F32)
    S_b = sb([128, 4, 128], BF16)
    tmpS = [sb([128, 128], F32) for _ in range(2)]
    h1 = sb([128, SBT, D], F32)
    TT = sb([128, 8, 512], F32)
    st6 = sb([128, SBT, 12], F32)
    mv = sb([128, SBT, 2], F32)
    lnr = sb([128, SBT, 2], F32)
    Bln = [Buf("ln%d" % j) for j in range(SBT)]
    Bconst = Buf("const")
    BS = [Buf("S%d" % h) for h in range(4)]
    BtmpS = [Buf("tmpS0"), Buf("tmpS1")]
    Bh1 = [Buf("h1_%d" % j) for j in range(SBT)]
    BT = [Buf("T%d" % i) for i in range(8)]

    region_lo = cur[0]

    xb = sb([128, D], BF16)
    hT = sb([128, 8, W], BF16)
    wblk = [sb([128, 8, 512], BF16) for _ in range(2)]
    hq_sb = sb([128, SBT, 512], BF16)
    hf_sb = sb([128, SBT, 512], F32)
    hi_sb = sb([128, SBT, 512], BF16)
    hg_sb = sb([128, SBT, 512], BF16)
    qT = sb([128, 4, W], BF16)
    mixT = sb([128, 8, W], BF16)
    qtl = sb([128, 512], BF16)
    ktl = sb([128, 512], BF16)
    qkT = sb([128, 8, 128], BF16)
    At = sb([128, 4, 128], BF16)
    mx = sb([128, 512], BF16)
    el = sb([128, 8], F32)
    ssq = sb([128, 4], F32)
    rstd4 = sb([128, 4], F32)
    Lp = [[sb([128, W], BF16) for _ in range(2)] for _ in range(2)]
    Aa = [[sb([128, W], BF16) for _ in range(2)] for _ in range(2)]
    r_sb = [sb([1, W], BF16) for _ in range(4)]
    Ef = [[sb([128, W], F32) for _ in range(3)] for _ in range(2)]
    Osb_t = sb([128, W], F32)
    rs_t = sb([128, W], F32)
    Xf = [sb([128, W], F32) for _ in range(2)]
    sqb = sb([128, W], BF16)
    Bxb = Buf("xb"); BhT = Buf("hT"); Bwblk = [Buf("wblk0"), Buf("wblk1")]
    Bhq = [Buf("hq%d" % j) for j in range(SBT)]
    Bhf = [Buf("hf%d" % j) for j in range(SBT)]
    Bhi = [Buf("hi%d" % j) for j in range(SBT)]
    Bhg = [Buf("hg%d" % j) for j in range(SBT)]
    BqT = Buf("qT")
    BmixT = [Buf("mixT%d" % k) for k in range(8)]
    Bqtl = Buf("qtl"); Bktl = Buf("ktl"); BqkT = Buf("qkT"); BAt = Buf("At"); Bmx = Buf("mx")
    Bel = Buf("el"); Bssq = Buf("ssq"); Brstd4 = Buf("rstd4")
    BLp = [[Buf("Lp%d%d" % (a_, b_)) for b_ in range(2)] for a_ in range(2)]; BAa = [[Buf("A%d%d" % (a_, b_)) for b_ in range(2)] for a_ in range(2)]; Br = [Buf("r%d" % i) for i in range(4)]; BEf = [[Buf("Ef%d%d" % (a_, b_)) for b_ in range(3)] for a_ in range(2)]; BXf = [Buf("Xf0"), Buf("Xf1")]; BOsb = Buf("Osb"); Brs = Buf("rs")
    Bsqb = Buf("sqb")
    p1_hi = cur[0]

    cur[0] = region_lo
    h1T_f = sb([128, 8, 128], F32)
    h1T_b = sb([128, 8, W], BF16)
    wslot = [sb([128, 8, 512], BF16) for _ in range(7)]
    lg = sb([128, SBT, 20], F32)
    rt = sb([128, SBT, 64], F32)
    lem = sb([128, SBT, 16], F32)
    top8 = sb([128, SBT, 8], F32)
    m1 = sb([128, SBT, 16], F32)
    m2 = sb([128, SBT, 16], F32)
    gates = sb([128, SBT, 16], F32)
    De = [sb([128, 128], BF16) for _ in range(2)]
    Gb = [sb([128, W], BF16) for _ in range(2)]
    s_sb = [sb([128, W], BF16) for _ in range(2)]
    u_sb = [sb([128, W], BF16) for _ in range(2)]
    hidg = [sb([128, 4, W], BF16) for _ in range(2)]
    Bh1Tf = Buf("h1Tf"); Bh1Tb = [Buf("h1Tb%d" % j) for j in range(SBT)]
    Bws = [Buf("ws%d" % i) for i in range(7)]
    Blg = [Buf("lg%d" % j) for j in range(SBT)]; Brt = [Buf("rt%d" % j) for j in range(SBT)]; Bgates = [Buf("gates%d" % j) for j in range(SBT)]
    BDe = [Buf("De0"), Buf("De1")]; BGb = [Buf("Gb0"), Buf("Gb1")]
    Bs = [Buf("s0"), Buf("s1")]; Bu = [Buf("u0"), Buf("u1")]; Bhidg = [Buf("hidg0"), Buf("hidg1")]
    p2_hi = cur[0]
    cur[0] = max(p1_hi, p2_hi)

    def mm(out, lhsT, rhs, start, stop, reads, writes):
        P.op("pe", lambda e: e.matmul(out, lhsT=lhsT, rhs=rhs, start=start, stop=stop, skip_group_check=True),
             reads, writes)

    def tr(out, in_, ident, reads, writes):
        P.op("pe", lambda e: e.transpose(out=out, in_=in_, identity=ident), reads, writes)

    def act(out, in_, func, reads, writes, **kw):
        P.op("act", lambda e: e.activation(out=out, in_=in_, func=func, **kw), reads, writes)

    def tt(eng, out, in0, in1, op, reads, writes):
        P.op(eng, lambda e: e.tensor_tensor(out=out, in0=in0, in1=in1, op=op), reads, writes)

    def ts(eng, out, in0, s1, op0, reads, writes, s2=None, op1=None):
        if op1 is None:
            P.op(eng, lambda e: e.tensor_scalar(out=out, in0=in0, scalar1=s1, scalar2=None, op0=op0), reads, writes)
        else:
            P.op(eng, lambda e: e.tensor_scalar(out=out, in0=in0, scalar1=s1, scalar2=s2, op0=op0, op1=op1),
                 reads, writes)

    def stt(out, in0, scalar, in1, op0, op1, reads, writes):
        P.op("dve", lambda e: e.scalar_tensor_tensor(out=out, in0=in0, scalar=scalar, in1=in1, op0=op0, op1=op1),
             reads, writes)

    def recip(out, in_, reads, writes):
        P.op("dve", lambda e: e.reciprocal(out=out, in_=in_), reads, writes)

    flip = [0]

    def evac(out, in_, reads, writes, scale=None):
        flip[0] ^= 1
        if flip[0]:
            if scale is None:
                act(out, in_, AF.Copy, reads, writes)
            else:
                act(out, in_, AF.Copy, reads, writes, scale=scale)
        else:
            if scale is None:
                P.op("dve", lambda e: e.tensor_copy(out=out, in_=in_), reads, writes)
            else:
                ts("dve", out, in_, scale, ALU.mult, reads, writes)

    def bcast_rows(src_ap_1xn, n):
        return bass.AP(tensor=src_ap_1xn.tensor, offset=src_ap_1xn.offset, ap=[[0, 128], [1, n]])

    Bwin = [Buf("w_in_bf%d" % i) for i in range(7)]; Bwout = Buf("w_out_bf"); Bexp = [Buf("exp%d" % e) for e in range(NE)]
    for cb in (1, 0, 2, 3, 6, 4, 5):
        for r in range(2):
            P.dma("pool", lambda e, r=r, cb=cb: e.dma_start(out=w_in_bf[r * 512:(r + 1) * 512, cb * 512:(cb + 1) * 512],
                                                            in_=w_in_d[r * 512:(r + 1) * 512, cb * 512:(cb + 1) * 512]),
                  writes=[Bwin[cb]])
    for r in range(4):
        P.dma("pool", lambda e, r=r: e.dma_start(out=w_out_bf[r * 256:(r + 1) * 256, :], in_=w_out_d[r * 256:(r + 1) * 256, :]),
              writes=[Bwout])
    for nm, ap in (("c_ident", ident_b), ("c_btri", btri_b), ("c_negtri", negtri_b), ("c_stri", stri_b),
                   ("c_bones", bones_b), ("c_ones", ones_b)):
        P.dma("pool", lambda e, nm=nm, ap=ap: e.dma_start(out=ap[:], in_=cst[nm]), writes=[Bconst])
    for nm, ap in (("c_ident", ident_f), ("c_btri", btri_f), ("c_ones", ones_f), ("c_cind", cind_f)):
        P.dma("sp", lambda e, nm=nm, ap=ap: e.dma_start(out=ap[:], in_=cst[nm]), writes=[Bconst])
    for src, dst, n in ((ln1g_d, ln1g, D), (ln1b_d, ln1b, D), (ln2g_d, ln2g, D), (ln2b_d, ln2b, D),
                        (hgg_d, hgg_rep, 512), (lbp_d[0:1, :], lb_rep, 512), (lbp_d[1:2, :], oml_rep, 512)):
        P.dma("sp", lambda e, src=src, dst=dst, n=n: e.dma_start(out=dst[:], in_=bcast_rows(src, n)), writes=[Bconst])
    P.dma("sp", lambda e: e.dma_start(out=sbgT[:], in_=sbg_d.rearrange("o (p q) -> q (o p)", q=128), allow_slow_non_contiguous=True), writes=[Bconst])
    P.dma("sp", lambda e: e.dma_start(out=w_r[:, :, 0:4], in_=wrg_d.rearrange("(k p) g -> p k g", p=128), allow_slow_non_contiguous=True), writes=[Bconst])
    P.dma("sp", lambda e: e.dma_start(out=w_r[:, :, 4:20], in_=wre_d.rearrange("(k p) g -> p k g", p=128), allow_slow_non_contiguous=True), writes=[Bconst])
    P.dma("sp", lambda e: e.dma_start(out=b_r[:, 0:4], in_=brg_d), writes=[Bconst])
    P.dma("sp", lambda e: e.dma_start(out=b_r[:, 4:20], in_=bre_d), writes=[Bconst])
    cast_next = [0]

    def issue_cast(n=1):
        for _ in range(n):
            e_ = cast_next[0]
            if e_ >= nexp:
                return
            cast_next[0] += 1
            thr = [Bexp[e_ - 2]] if e_ >= 2 else [Bwout, Bconst]
            for (src, dst) in ((w1_d, w1_bf), (w3_d, w3_bf)):
                for hh in range(2):
                    P.dma("pool", lambda e, src=src, dst=dst, e_=e_, hh=hh: e.dma_start(
                        out=dst[e_, hh * 512:(hh + 1) * 512, :], in_=src[e_, hh * 512:(hh + 1) * 512, :]),
                        reads=thr, writes=[Bexp[e_]])
            for hh in range(2):
                P.dma("pool", lambda e, e_=e_, hh=hh: e.dma_start(
                    out=w2_bf[e_, hh * 256:(hh + 1) * 256, :], in_=w2_d[e_, hh * 256:(hh + 1) * 256, :]),
                    reads=thr, writes=[Bexp[e_]])

    issue_cast(NE)
    tt("dve", oml_rep[:], oml_rep[:], lb_rep[:], ALU.subtract, [Bconst], [Bconst])
    act(oml_rep[:], oml_rep[:], AF.Exp, [Bconst], [Bconst])
    ts("dve", lb_rep[:], oml_rep[:], 1.0, ALU.add, [Bconst], [Bconst])
    recip(lb_rep[:], lb_rep[:], [Bconst], [Bconst])
    tt("dve", oml_rep[:], oml_rep[:], lb_rep[:], ALU.mult, [Bconst], [Bconst])
    P.op("dve", lambda e: e.memset(S_f[:], 0.0), [], BS)
    P.op("dve", lambda e: e.memset(S_b[:], 0.0), [], BS)

    def Tf(i, n=512):
        return TT[:, i, 0:n]

    def load_x_rows(eng, dst, i, Bdst):
        if i == 0:
            P.dma(eng, lambda e: e.dma_start(out=dst[0:16, :], in_=meta_d[:, :]), writes=[Bdst])
            P.dma(eng, lambda e: e.dma_start(out=dst[16:128, :], in_=x_d[0:112, :]), writes=[Bdst])
        elif i < NT - 1:
            P.dma(eng, lambda e: e.dma_start(out=dst[:, :], in_=x_d[128 * i - 16:128 * i + 112, :]), writes=[Bdst])
        else:
            P.op("dve", lambda e: e.memset(dst[:, :], 0.0), [], [Bdst])
            P.dma(eng, lambda e: e.dma_start(out=dst[0:16, :], in_=x_d[SEQ - 16:SEQ, :]), writes=[Bdst])

    wctr = [0]

    def load_wblk(src3, dep):
        i = wctr[0] % 2
        wctr[0] += 1
        P.dma("sp", lambda e: e.dma_start(out=wblk[i][:], in_=src3), reads=[dep], writes=[Bwblk[i]])
        return i

    bankrr = [0]

    def next_bank(lo=0, hi=6):
        b = lo + bankrr[0] % (hi - lo)
        bankrr[0] += 1
        return b

    pending_ln2 = [[]]

    def flush_ln2(n):
        lst = pending_ln2[0]
        for _ in range(min(n, len(lst))):
            P.play(lst.pop(0))

    for Q in range(min(NSB, maxq)):
        t0 = Q * SBT
        PL = "dve" if Q == 0 else "pool"
        tok0 = t0 * 128
        for j in range(SBT):
            xst = TT[:, 6:8, :].rearrange("p a b -> p (a b)")
            load_x_rows("sp", xst, t0 + j, BT[6])
            act(xb[:, :], xst, AF.Copy, [BT[6]], [Bxb])
            for k in range(8):
                tr(psb[7][:, k * 128:(k + 1) * 128], xb[:, k * 128:(k + 1) * 128], ident_b[:], [Bxb, Bconst], [PB[7]])
            evac(hT[:, :, j * 128:(j + 1) * 128], psb[7][:, :].rearrange("p (k t) -> p k t", k=8), [PB[7]], [BhT])
        for cb in (1, 0, 2, 3, 6, 4, 5):
            wi = load_wblk(w_in_bf[:, cb * 512:(cb + 1) * 512].rearrange("(k p) c -> p k c", p=128), Bwin[cb])
            if cb in (4, 5):
                for p in range(4):
                    b = next_bank()
                    for k in range(8):
                        mm(psf[b][:, 0:W], wblk[wi][:, k, p * 128:(p + 1) * 128], hT[:, k, :], k == 0, k == 7,
                           [Bwblk[wi], BhT], [PB[b]])
                    if cb == 4:
                        evac(qT[:, p, :], psf[b][:, 0:W], [PB[b]], [BqT], scale=0.125)
                    else:
                        evac(kT[:, p, tok0:tok0 + W], psf[b][:, 0:W], [PB[b]], [BkT[Q]])
                    flush_ln2(2)
            else:
                for j in range(SBT):
                    b = next_bank()
                    for k in range(8):
                        mm(psf[b][:, :], hT[:, k, j * 128:(j + 1) * 128], wblk[wi][:, k, :], k == 0, k == 7,
                           [Bwblk[wi], BhT], [PB[b]])
                    if cb == 0:
                        evac(hq_sb[:, j, :], psf[b][:, :], [PB[b]], [Bhq[j]])
                    elif cb == 1:
                        evac(hf_sb[:, j, :], psf[b][:, :], [PB[b]], [Bhf[j]])
                    elif cb == 2:
                        evac(hi_sb[:, j, :], psf[b][:, :], [PB[b]], [Bhi[j]])
                    elif cb == 3:
                        evac(hg_sb[:, j, :], psf[b][:, :], [PB[b]], [Bhg[j]])
                    else:
                        evac(vv[:, t0 + j, :], psf[b][:, :], [PB[b]], [Bv[Q]])
                    flush_ln2(2)

        flush_ln2(10 ** 6)
        if stop == '1a':
            continue
        deferred = []
        P.defer = deferred
        for j in range(SBT):
            t1, t2, t3, t4, t5, t6, t7 = (Tf(i) for i in range(7))
            B1, B2, B3, B4, B5, B6, B7 = BT[0:7]
            if Q == 0:
                issue_cast(1)
            act(t1, hf_sb[:, j, :], AF.Sigmoid, [Bhf[j]], [B1])
            deferred.append("GLUE")
            act(t6, hq_sb[:, j, :], AF.Sigmoid, [Bhq[j]], [B6])
            deferred.append("GLUE")
            act(t7, hg_sb[:, j, :], AF.Sigmoid, [Bhg[j]], [B7])
            tt("dve", t1, t1, oml_rep[:], ALU.mult, [B1, Bconst], [B1])
            tt("dve", t6, t6, hq_sb[:, j, :], ALU.mult, [B6, Bhq[j]], [B6])
            tt("dve", t7, t7, hgg_rep[:], ALU.mult, [B7, Bconst], [B7])
            tt("dve", t1, t1, lb_rep[:], ALU.add, [B1, Bconst], [B1])
            act(t2, t1, AF.Ln, [B1], [B2])
            ts("dve", t3, t1, -1.0, ALU.mult, [B1], [B3], s2=1.0, op1=ALU.add)
            o_sb = Tf(7)
            for h in range(4):
                mm(psf[7][:, 2 * h:2 * h + 2], t2[:, h * 128:(h + 1) * 128], cind_f[:], h == 0, h == 3,
                   [Bconst, B2], [PB[7]])
            act(el[:], psf[7][:, 0:8], AF.Exp, [PB[7]], [Bel])
            deferred.append(None)
            mm(psf[7][:, :], btri_f[:], t2, True, True, [Bconst, B2], [PB[7]])
            act(t4, psf[7][:, :], AF.Exp, [PB[7]], [B4])
            act(t5, psf[7][:, :], AF.Exp, [PB[7]], [B5], scale=-1.0)
            deferred.append(None)
            tt("dve", qtl[:], t6, t4, ALU.mult, [B6, B4], [Bqtl])
            tt("dve", ktl[:], t3, t5, ALU.mult, [B3, B5], [Bktl])
            for h in range(4):
                tr(psb[7][:, h * 128:(h + 1) * 128], qtl[:, h * 128:(h + 1) * 128], ident_b[:], [Bqtl, Bconst], [PB[7]])
            for h in range(4):
                tr(psb[7][:, (4 + h) * 128:(5 + h) * 128], ktl[:, h * 128:(h + 1) * 128], ident_b[:], [Bktl, Bconst], [PB[7]])
            evac(qkT[:, :, :], psb[7][:, :].rearrange("p (k t) -> p k t", k=8), [PB[7]], [BqkT])
            deferred.append(None)
            for h in range(4):
                mm(psf[7][:, h * 128:(h + 1) * 128], qkT[:, 4 + h, :], qkT[:, h, :], h == 0, h == 3, [BqkT], [PB[7]])
            btri_bc = bass.AP(tensor=btri_b, offset=0, ap=[[128, 128], [0, 4], [1, 128]])
            tt("dve", At[:, :, :], psf[7][:, :].rearrange("p (h t) -> p h t", h=4), btri_bc, ALU.mult,
               [PB[7], Bconst], [BAt])
            deferred.append(None)
            for h in range(4):
                hs = slice(h * 128, (h + 1) * 128)
                mm(psf[7][:, hs], At[:, h, :], hi_sb[:, j, hs], h == 0, h == 3, [BAt, Bhi[j]], [PB[7]])
            evac(o_sb, psf[7][:, :], [PB[7]], [BT[7]])
            deferred.append(None)
            for c in range(2):
                rows = slice(64 * c, 64 * c + 64)
                for h in range(4):
                    hs = slice(h * 128, (h + 1) * 128)
                    mm(psf[7][rows, hs], qkT[:, h, rows], S_b[:, h, :], h == 0, h == 3, [BqkT, BS[h]], [PB[7]])
                tt("dve", o_sb[rows, :], psf[7][rows, :], o_sb[rows, :], ALU.add, [PB[7], BT[7]], [BT[7]])
                deferred.append(None)
                for h in range(4):
                    hs = slice(h * 128, (h + 1) * 128)
                    mm(psf[7][:, hs], ktl[rows, hs], hi_sb[rows, j, hs], h == 0, h == 3, [Bktl, Bhi[j]], [PB[7]])
                S_f2 = S_f[:, :, :].rearrange("p h d -> p (h d)")
                tt("dve", t2, psf[7][:, :], S_f2, ALU.add, [PB[7]] + BS, [B2])
                deferred.append(None)
                el_bc = bass.AP(tensor=el, offset=c, ap=[[8, 128], [2, 4], [0, 128]])
                t2v = t2.rearrange("p (h d) -> p h d", h=4)
                tt("dve", S_b[:, :, :], t2v, el_bc, ALU.mult, [B2, Bel], BS)
                tt("dve", S_f[:, :, :], t2v, el_bc, ALU.mult, [B2, Bel], BS)
            P.op(PL, lambda e: e.memset(ssq[:], 0.0), [], [Bssq])
            for h in range(4):
                hs = slice(h * 128, (h + 1) * 128)
                act(t1[:, hs], o_sb[:, hs], AF.Square, [BT[7]], [B1, Bssq], accum_out=ssq[:, h:h + 1])
            ts("dve", rstd4[:], ssq[:], 1.0 / 128.0, ALU.mult, [Bssq], [Brstd4], s2=RMS_EPS, op1=ALU.add)
            act(rstd4[:], rstd4[:], AF.Ln, [Brstd4], [Brstd4])
            act(rstd4[:], rstd4[:], AF.Exp, [Brstd4], [Brstd4], scale=-0.5)
            for h in range(4):
                hs = slice(h * 128, (h + 1) * 128)
                stt(mx[:, hs], o_sb[:, hs], rstd4[:, h:h + 1], t7[:, hs], ALU.mult, ALU.mult,
                    [BT[7], Brstd4, B7], [Bmx])
            for h in range(4):
                tr(psb[7][:, h * 128:(h + 1) * 128], mx[:, h * 128:(h + 1) * 128], ident_b[:], [Bmx, Bconst], [PB[7]])
            evac(mixT[:, 0:4, j * 128:(j + 1) * 128], psb[7][:, 0:512].rearrange("p (k t) -> p k t", k=4),
                 [PB[7]], BmixT[0:4])
            deferred.append(None)

        if stop == '1b':
            for it_ in deferred:
                if it_ is not None and it_ != "GLUE":
                    P.play(it_)
            continue
        P.defer = None
        jmax = t0 + SBT - 1
        groups = [(p, j) for p in range(4) for j in range(jmax, -1, -1)]
        ng = len(groups)
        PRS = (slice(0, 64), slice(64, 128))

        def ginfo(g):
            p, j = groups[g]
            c0 = 128 * max(0, j - t0)
            return dict(p=p, j=j, c0=c0, cs=slice(c0, W), first=(j == jmax), Qi=j // SBT, ob=4,
                        e3=g % 3, s2=g % 2, ri=[(p % 2) * 2, (p % 2) * 2 + 1],
                        cb=[2, 3] if g % 2 == 0 else [5, 6])

        def sA(g):
            t = ginfo(g)
            cs = t["cs"]
            if t["first"]:
                for hf_ in range(2):
                    P.op(PL, lambda e, ri=t["ri"][hf_]: e.memset(r_sb[ri][:], 0.0), [], [Br[t["ri"][hf_]]])
            for hf_ in range(2):
                mm(psf[hf_][:, cs], kT[PRS[hf_], t["p"], t["j"] * 128:(t["j"] + 1) * 128], qT[PRS[hf_], t["p"], cs], True, True,
                   [BkT[t["Qi"]], BqT], [PB[0], PB[1]] if hf_ == 0 else [PB[1]])

        def sB(g):
            t = ginfo(g)
            cs, c0, e3, s2 = t["cs"], t["c0"], t["e3"], t["s2"]
            for hf_ in range(2):
                act(Ef[hf_][e3][:, cs], psf[hf_][:, cs], AF.Exp, [PB[hf_]], [BEf[hf_][e3]])
            for hf_ in range(2):
                act(Lp[hf_][s2][:, cs], Ef[hf_][e3][:, cs], AF.Ln, [BEf[hf_][e3]], [BLp[hf_][s2]], bias=1.0)
            if t["j"] >= t0:
                for hf_ in range(2):
                    tt("dve", Lp[hf_][s2][:, c0:c0 + 128], Lp[hf_][s2][:, c0:c0 + 128], stri_b[:], ALU.mult,
                       [BLp[hf_][s2], Bconst], [BLp[hf_][s2]])

        def sC(g):
            t = ginfo(g)
            cs, s2, cb = t["cs"], t["s2"], t["cb"]
            for hf_ in range(2):
                mm(psf[cb[hf_]][:, cs], negtri_b[:], Lp[hf_][s2][:, cs], True, t["first"],
                   [Bconst, BLp[0][s2], BLp[1][s2]] if hf_ == 0 else [Bconst, BLp[1][s2]],
                   [PB[cb[0]], PB[cb[1]]] if hf_ == 0 else [PB[cb[1]]])
            if not t["first"]:
                for hf_ in range(2):
                    ri = t["ri"][hf_]
                    mm(psf[cb[hf_]][:, cs], ones_b[0:1, :], r_sb[ri][0:1, cs], False, True,
                       [Bconst, Br[t["ri"][0]], Br[t["ri"][1]]] if hf_ == 0 else [Bconst, Br[ri]], [PB[cb[hf_]]])

        def sD(g):
            t = ginfo(g)
            cs, c0, e3, s2, cb = t["cs"], t["c0"], t["e3"], t["s2"], t["cb"]
            for hf_ in range(2):
                ri = t["ri"][hf_]
                if t["j"] > 0:
                    P.op("dve", lambda e, ri=ri, hf_=hf_: e.tensor_copy(out=r_sb[ri][0:1, cs], in_=psf[cb[hf_]][0:1, cs]),
                         [PB[cb[hf_]]], [Br[ri]])
                act(Xf[hf_][:, cs], psf[cb[hf_]][:, cs], AF.Exp, [PB[cb[hf_]], Br[ri]], [BXf[hf_]])
            for hf_ in range(2):
                eng_ = "dve" if hf_ == 0 else PL
                tt(eng_, Aa[hf_][s2][:, cs], Ef[hf_][e3][:, cs], Xf[hf_][:, cs], ALU.mult, [BEf[hf_][e3], BXf[hf_]], [BAa[hf_][s2]])
                if t["j"] >= t0:
                    tt(eng_, Aa[hf_][s2][:, c0:c0 + 128], Aa[hf_][s2][:, c0:c0 + 128], stri_b[:], ALU.mult,
                       [BAa[hf_][s2], Bconst], [BAa[hf_][s2]])

        def sF(g):
            t = ginfo(g)
            cs, ob, p, s2 = t["cs"], t["ob"], t["p"], t["s2"]
            for hf_ in range(2):
                h = 2 * p + hf_
                mm(psf[ob][PRS[hf_], cs], vv[:, t["j"], h * 64:(h + 1) * 64], Aa[hf_][s2][:, cs], t["first"], t["j"] == 0,
                   [Bv[t["Qi"]], BAa[0][s2], BAa[1][s2]] if hf_ == 0 else [Bv[t["Qi"]], BAa[1][s2]], [PB[ob]])
            if t["j"] == 0:
                Osb = Osb_t[:, :]
                rs = rs_t[:, :]
                act(Osb, psf[ob][:, 0:W], AF.Copy, [PB[ob]], [BOsb])
                act(sqb[:], psf[ob][:, 0:W], AF.Square, [PB[ob]], [Bsqb])
                mm(psf[7][:, 0:W], bones_b[:], sqb[:], True, True, [Bconst, Bsqb], [PB[7]])
                ts("dve", rs, psf[7][:, 0:W], 1.0 / 64.0, ALU.mult, [PB[7]], [Brs], s2=RMS_EPS, op1=ALU.add)
                act(rs, rs, AF.Ln, [Brs], [Brs])
                act(rs, rs, AF.Exp, [Brs], [Brs], scale=-0.5)
                stt(mixT[:, 4 + p, :], Osb, sbgT[:, p:p + 1], rs, ALU.mult, ALU.mult, [BOsb, Brs, Bconst], [BmixT[4 + p]])

        stages = ((sF, 4), (sD, 3), (sC, 2), (sB, 1), (sA, 0))
        nit = ng + 4
        ndef = sum(1 for x in deferred if x is not None and x != "GLUE")
        per_it = -(-ndef // nit)
        in_b7 = [False] * len(deferred)
        open_ = False
        for i_, item in enumerate(deferred):
            if item is None:
                open_ = False
                continue
            if item == "GLUE":
                continue
            in_b7[i_] = open_
            if PB[7] in item[4]:
                open_ = True
        dpos = 0
        for it in range(nit):
            if Q == 0 and it % 2 == 0:
                issue_cast(1)
            for fn_, lag in stages:
                g = it - lag
                if 0 <= g < ng:
                    fn_(g)
            played = 0
            glue = False
            while dpos < len(deferred):
                item = deferred[dpos]
                if item is None:
                    dpos += 1
                    if played >= per_it:
                        break
                    continue
                if item == "GLUE":
                    dpos += 1
                    glue = True
                    continue
                if played >= per_it and not in_b7[dpos] and not glue:
                    break
                P.play(item)
                glue = False
                played += 1
                dpos += 1
        while dpos < len(deferred):
            if deferred[dpos] is not None and deferred[dpos] != "GLUE":
                P.play(deferred[dpos])
            dpos += 1

        if dbg:
            for k in range(8):
                P.dma("sp", lambda e, k=k: e.dma_start(out=dbg_mixT[k, :, tok0:tok0 + W], in_=mixT[:, k, :]), reads=[BmixT[k]])

        if stop == '1c':
            continue
        def layer_norm3(pres, Bpres, nrms, Bnrms, g_rep, b_rep, dsts, Bdsts):
            J = range(len(pres))
            for c in range(2):
                for j in J:
                    P.op("dve", lambda e, c=c, j=j: e.bn_stats(out=st6[:, j, c * 6:(c + 1) * 6], in_=pres[j][:, c * 512:(c + 1) * 512]),
                         Bpres[j], [Bln[j]])
            for j in J:
                P.op("dve", lambda e, j=j: e.bn_aggr(out=mv[:, j, :], in_=st6[:, j, :]), [Bln[j]], [Bln[j]])
            for j in J:
                ts("dve", lnr[:, j, 0:1], mv[:, j, 1:2], LN_EPS, ALU.add, [Bln[j]], [Bln[j]])
            for j in J:
                act(lnr[:, j, 0:1], lnr[:, j, 0:1], AF.Ln, [Bln[j]], [Bln[j]])
            for j in J:
                act(lnr[:, j, 0:1], lnr[:, j, 0:1], AF.Exp, [Bln[j]], [Bln[j]], scale=-0.5)
            for j in J:
                stt(lnr[:, j, 1:2], mv[:, j, 0:1], -1.0, lnr[:, j, 0:1], ALU.mult, ALU.mult, [Bln[j]], [Bln[j]])
            for j in J:
                act(nrms[j], pres[j], AF.Identity, Bpres[j] + [Bln[j]], Bnrms[j], scale=lnr[:, j, 0:1], bias=lnr[:, j, 1:2])
            for j in J:
                tt("dve", nrms[j], nrms[j], g_rep[:], ALU.mult, Bnrms[j] + [Bconst], Bnrms[j])
            for j in J:
                tt(PL, dsts[j], nrms[j], b_rep[:], ALU.add, Bnrms[j] + [Bconst], Bdsts[j])

        xr = [TT[:, 2 * j:2 * j + 2, :].rearrange("p a b -> p (a b)") for j in range(SBT)]
        Bxr = [BT[2 * j:2 * j + 2] for j in range(SBT)]
        for j in range(SBT):
            i = t0 + j
            xres = xr[j]
            Bx_ = Bxr[j]
            if i == 0:
                P.dma("sp", lambda e, xres=xres: e.dma_start(out=xres[0:16, :], in_=meta_d[:, :]), writes=Bx_)
                P.dma("sp", lambda e, xres=xres: e.dma_start(out=xres[16:128, :], in_=x_d[0:112, :]), writes=Bx_)
            elif i < NT - 1:
                P.dma("sp", lambda e, i=i, xres=xres: e.dma_start(out=xres[:, :], in_=x_d[128 * i - 16:128 * i + 112, :]), writes=Bx_)
            else:
                P.op(PL, lambda e, xres=xres: e.memset(xres[:, :], 0.0), [], Bx_)
                P.dma("sp", lambda e, xres=xres: e.dma_start(out=xres[0:16, :], in_=x_d[SEQ - 16:SEQ, :]), writes=Bx_)
        for half in range(2):
            wi = load_wblk(w_out_bf[:, half * 512:(half + 1) * 512].rearrange("(k p) c -> p k c", p=128), Bwout)
            hs = slice(half * 512, (half + 1) * 512)
            for j in range(SBT):
                b = next_bank(0, 6)
                for k in range(8):
                    mm(psf[b][:, :], mixT[:, k, j * 128:(j + 1) * 128], wblk[wi][:, k, :], k == 0, k == 7,
                       [BmixT[k], Bwblk[wi]], [PB[b]])
                stt(xr[j][:, hs], xr[j][:, hs], ALPHA, psf[b][:, :], ALU.mult, ALU.add, Bxr[j] + [PB[b]], Bxr[j])
        layer_norm3(xr, Bxr, xr, Bxr, ln1g, ln1b, [h1[:, j, :] for j in range(SBT)], [[Bh1[j]] for j in range(SBT)])
        if dbg:
            for j in range(SBT):
                i = t0 + j
                P.dma("sp", lambda e, i=i, j=j: e.dma_start(out=dbg_h1[i * 128:(i + 1) * 128, :], in_=h1[:, j, :]), reads=[Bh1[j]])

        if stop == '1d':
            continue
        if Q == 0:
            issue_cast(NE)
        P.barrier()
        for j in range(SBT):
            for half in range(2):
                for k4 in range(4):
                    k = half * 4 + k4
                    mm(psf[half][:, k4 * 128:(k4 + 1) * 128], h1[:, j, k * 128:(k + 1) * 128], ident_f[:], k4 == 0, k4 == 3,
                       [Bh1[j], Bconst], [PB[half]])
                act(h1T_f[:, half * 4:half * 4 + 4, :], psf[half][:, :].rearrange("p (k t) -> p k t", k=4), AF.Copy,
                    [PB[half]], [Bh1Tf])
                act(h1T_b[:, half * 4:half * 4 + 4, j * 128:(j + 1) * 128], h1T_f[:, half * 4:half * 4 + 4, :], AF.Copy,
                    [Bh1Tf], [Bh1Tb[j]])
            ts(PL, h1[:, j, :], h1[:, j, :], ALPHA, ALU.mult, [Bh1[j]], [Bh1[j]])
            for k in range(8):
                mm(psf[2][:, 0:20], h1T_f[:, k, :], w_r[:, k, :], k == 0, False, [Bh1Tf, Bconst], [PB[2]])
            mm(psf[2][:, 0:20], ones_f[0:1, :], b_r[0:1, :], False, True, [Bconst], [PB[2]])
            P.op("dve", lambda e, j=j: e.tensor_copy(out=lg[:, j, :], in_=psf[2][:, 0:20]), [PB[2]], [Blg[j]])
        if stop in ('2a0', '2a1'):
            continue

        def rc(j, i):
            return rt[:, j, i:i + 1]
        J3 = range(SBT)
        RB = lambda j: [Blg[j], Brt[j]]
        for j in J3:
            P.op(PL, lambda e, j=j: e.memset(rt[:, j, 0:16], 0.0), [], [Brt[j]])
        for j in J3:
            P.op("dve", lambda e, j=j: e.reduce_max(out=rc(j, 0), in_=lg[:, j, 0:4], axis=AX.X), RB(j), [Brt[j]])
        for j in J3:
            ts("dve", rc(j, 1), rc(j, 0), -1.0, ALU.mult, RB(j), [Brt[j]])
        for j in J3:
            ts("dve", rt[:, j, 16:20], lg[:, j, 0:4], rc(j, 0), ALU.is_equal, RB(j), [Brt[j]])
        for j in J3:
            act(rt[:, j, 24:28], lg[:, j, 0:4], AF.Exp, RB(j), [Brt[j]], bias=rc(j, 1), scale=1.0, accum_out=rc(j, 2))
        for j in J3:
            ts("dve", rt[:, j, 20:24], rt[:, j, 16:20], BIG, ALU.mult, RB(j), [Brt[j]], s2=-BIG, op1=ALU.add)
        for j in J3:
            pen = rt[:, j, 20:24]
            pen_bc = bass.AP(tensor=pen.tensor, offset=pen.offset, ap=[list(pen.ap[0]), [1, 4], [0, 4]])
            tt("dve", lem[:, j, :].rearrange("p (g e) -> p g e", g=4), lg[:, j, 4:20].rearrange("p (g e) -> p g e", g=4),
               pen_bc, ALU.add, RB(j), [Brt[j]])
        for j in J3:
            recip(rc(j, 3), rc(j, 2), RB(j), [Brt[j]])
        for j in J3:
            P.op("dve", lambda e, j=j: e.max(out=top8[:, j, :], in_=lem[:, j, :]), [Brt[j]], [Brt[j]])
        for j in J3:
            ts("dve", m1[:, j, :], lem[:, j, :], top8[:, j, 0:1], ALU.is_equal, [Brt[j]], [Brt[j]])
        for j in J3:
            ts("dve", m2[:, j, :], lem[:, j, :], top8[:, j, 1:2], ALU.is_equal, [Brt[j]], [Brt[j]])
        for j in J3:
            tt("dve", rc(j, 4), top8[:, j, 1:2], top8[:, j, 0:1], ALU.subtract, [Brt[j]], [Brt[j]])
        for j in J3:
            act(rc(j, 5), rc(j, 4), AF.Exp, [Brt[j]], [Brt[j]])
        for j in J3:
            ts("dve", rc(j, 6), rc(j, 5), 1.0, ALU.add, [Brt[j]], [Brt[j]])
        for j in J3:
            recip(rc(j, 6), rc(j, 6), [Brt[j]], [Brt[j]])
        for j in J3:
            tt("dve", rc(j, 7), rc(j, 5), rc(j, 6), ALU.mult, [Brt[j]], [Brt[j]])
        for j in J3:
            tt("dve", rc(j, 8), rc(j, 6), rc(j, 3), ALU.mult, [Brt[j]], [Brt[j]])
        for j in J3:
            tt("dve", rc(j, 9), rc(j, 7), rc(j, 3), ALU.mult, [Brt[j]], [Brt[j]])
        for j in J3:
            ts("dve", m1[:, j, :], m1[:, j, :], rc(j, 8), ALU.mult, [Brt[j]], [Brt[j]])
        for j in J3:
            stt(gates[:, j, :], m2[:, j, :], rc(j, 9), m1[:, j, :], ALU.mult, ALU.add, [Brt[j]], [Bgates[j]])
        if stop == '2a':
            continue

        slot = [0]

        def load_expert(e_):
            idx = []
            for src in (w1_bf[e_].rearrange("(k p) c -> p k c", p=128), w3_bf[e_].rearrange("(k p) c -> p k c", p=128)):
                i = slot[0] % 7
                slot[0] += 1
                P.dma("sp", lambda e, i=i, src=src: e.dma_start(out=wslot[i][:], in_=src), reads=[Bexp[e_]], writes=[Bws[i]])
                idx.append(i)
            i = slot[0] % 7
            slot[0] += 1
            dst = wslot[i][:, :, :].rearrange("p a b -> p (a b)").rearrange("p (c n) -> p c n", c=4)
            src = w2_bf[e_].rearrange("(c p) n -> p c n", p=128)
            P.dma("sp", lambda e, dst=dst, src=src: e.dma_start(out=dst, in_=src), reads=[Bexp[e_]], writes=[Bws[i]])
            idx.append(i)
            return idx

        def gate_bcast(e_):
            gi = e_ % 2
            for j in range(SBT):
                di = j % 2
                ts("dve", De[di][:], ident_b[:], gates[:, j, e_:e_ + 1], ALU.mult, [Bconst, Bgates[j]], [BDe[di]])
                mm(psf[3][:, j * 128:(j + 1) * 128], ones_b[:], De[di][:], j == 0, j == SBT - 1, [Bconst, BDe[di]], [PB[3]])
            act(Gb[gi][:], psf[3][:, 0:W], AF.Copy, [PB[3]], [BGb[gi]])

        pbank = [0]

        def hidden_chunk(e_, c, i1, i3, part="AB"):
            gi = e_ % 2
            hi_ = e_ % 2
            if part == "B":
                bi = c % 2
                tt(PL, hidg[hi_][:, c, :], u_sb[bi][:], Gb[gi][:], ALU.mult, [Bu[bi], BGb[gi]], [Bhidg[hi_]])
                return
            bi = pbank[0] % 2
            pbank[0] += 1
            b1, b3 = (4, 5) if bi == 0 else (6, 7)
            for k in range(8):
                mm(psf[b1][:, 0:W], wslot[i1][:, k, c * 128:(c + 1) * 128], h1T_b[:, k, :], k == 0, k == 7,
                   [Bws[i1]] + Bh1Tb, [PB[b1]])
            for k in range(8):
                mm(psf[b3][:, 0:W], wslot[i3][:, k, c * 128:(c + 1) * 128], h1T_b[:, k, :], k == 0, k == 7,
                   [Bws[i3]] + Bh1Tb, [PB[b3]])
            act(s_sb[bi][:], psf[b1][:, 0:W], AF.Silu, [PB[b1]], [Bs[bi]])
            tt("dve", u_sb[bi][:], psf[b3][:, 0:W], s_sb[bi][:], ALU.mult, [PB[b3], Bs[bi]], [Bu[bi]])
            if part == "A":
                return
            tt(PL, hidg[hi_][:, c, :], u_sb[bi][:], Gb[gi][:], ALU.mult, [Bu[bi], BGb[gi]], [Bhidg[hi_]])

        def down_proj(e_, i2):
            hi_ = e_ % 2
            w2v = wslot[i2][:, :, :].rearrange("p a b -> p (a b)")
            for j in range(SBT):
                for half in range(2):
                    b = next_bank(0, 3)
                    for c in range(4):
                        mm(psf[b][:, :], hidg[hi_][:, c, j * 128:(j + 1) * 128],
                           w2v[:, c * 1024 + half * 512:c * 1024 + (half + 1) * 512], c == 0, c == 3,
                           [Bhidg[hi_], Bws[i2]], [PB[b]])
                    hs = slice(half * 512, (half + 1) * 512)
                    tt("dve", h1[:, j, hs], psf[b][:, :], h1[:, j, hs], ALU.add, [PB[b], Bh1[j]], [Bh1[j]])

        widx = {0: load_expert(0)}
        pbank[0] = 0
        for e_ in range(NE):
            i1, i3, i2 = widx[e_]
            hidden_chunk(e_, 0, i1, i3, "A" if e_ == 0 else "AB")
            if e_ + 1 < NE:
                widx[e_ + 1] = load_expert(e_ + 1)
            if e_ > 0:
                down_proj(e_ - 1, widx[e_ - 1][2])
            if e_ == 0:
                hidden_chunk(e_, 1, i1, i3, "A")
                gate_bcast(0)
                hidden_chunk(e_, 0, i1, i3, "B")
                hidden_chunk(e_, 1, i1, i3, "B")
            if e_ + 1 < NE:
                gate_bcast(e_ + 1)
            for c in range(2 if e_ == 0 else 1, 4):
                hidden_chunk(e_, c, i1, i3)
        down_proj(NE - 1, widx[NE - 1][2])
        if stop == '2b':
            continue
        P.barrier()
        ln2_def = []
        P.defer = ln2_def
        nr = [TT[:, 2 * j:2 * j + 2, :].rearrange("p a b -> p (a b)") for j in range(SBT)]
        Bnr = [BT[2 * j:2 * j + 2] for j in range(SBT)]
        layer_norm3([h1[:, j, :] for j in range(SBT)], [[Bh1[j]] for j in range(SBT)], nr, Bnr, ln2g, ln2b, nr, Bnr)
        for j in range(SBT):
            i = t0 + j
            ot = nr[j]
            if i == 0:
                P.dma("sp", lambda e, ot=ot: e.dma_start(out=out_d[0:112, :], in_=ot[16:128, :]), reads=Bnr[j])
            elif i < NT - 1:
                P.dma("sp", lambda e, ot=ot, i=i: e.dma_start(out=out_d[128 * i - 16:128 * i + 112, :], in_=ot[:, :]), reads=Bnr[j])
            else:
                P.dma("sp", lambda e, ot=ot: e.dma_start(out=out_d[SEQ - 16:SEQ, :], in_=ot[0:16, :]), reads=Bnr[j])
        P.defer = None
        pending_ln2[0] = ln2_def
        if Q == min(NSB, maxq) - 1:
            flush_ln2(len(ln2_def))

    P.emit()
    return nc


_CACHE = {}


def _prep_inputs(inputs, b):
    f = lambda a: np.ascontiguousarray(np.asarray(a, dtype=np.float32))
    m = {
        "x": f(inputs["x"][b]),
        "meta_tokens": f(inputs["meta_tokens"]),
        "w_in": f(inputs["w_in"][0]),
        "hg_lower_bound": f(inputs["hg_lower_bound"]),
        "hg_norm_g": f(inputs["hg_norm_g"]),
        "sb_norm_g": f(inputs["sb_norm_g"]),
        "w_out": f(inputs["w_out"][0]),
        "ln1_g": f(inputs["ln1_g"]), "ln1_b": f(inputs["ln1_b"]),
        "w_router_group": f(inputs["w_router_group"][0]),
        "b_router_group": f(inputs["b_router_group"]),
        "w_router_expert": f(np.asarray(inputs["w_router_expert"][0]).reshape(D, 16)),
        "b_router_expert": f(np.asarray(inputs["b_router_expert"]).reshape(1, 16)),
        "w_exp_gate": f(inputs["w_exp_gate"][0]),
        "w_exp_up": f(inputs["w_exp_up"][0]),
        "w_exp_down": f(inputs["w_exp_down"][0]),
        "ln2_g": f(inputs["ln2_g"]), "ln2_b": f(inputs["ln2_b"]),
    }
    m.update(host_consts())
    return m


def kernel(**inputs):
    nc = bass.Bass("TRN2", target_bir_lowering=False)
    build(nc)
    n = 8
    in_maps = [_prep_inputs(inputs, b) for b in range(n)]
    res = run_bass_kernel_spmd(nc, in_maps, core_ids=list(range(n)))
    return np.stack([np.asarray(r["out"], dtype=np.float32) for r in res.results], axis=0)
```

```python
import os
import numpy as np
import concourse.bass as bass
import concourse.mybir as mybir
from concourse.bass_utils import run_bass_kernel_spmd

F32 = mybir.dt.float32
BF16 = mybir.dt.bfloat16
AF = mybir.ActivationFunctionType
ALU = mybir.AluOpType
AX = mybir.AxisListType

D = 1024
SEQ = 4096
NMETA = 16
L = SEQ + NMETA
NT = 33
LP = NT * 128
SBT = 3
NSB = NT // SBT
W = SBT * 128
NE = 16
ALPHA = 2 ** 0.25
LN_EPS = 1e-5
RMS_EPS = 1e-6
BIG = 1.0e4
SAME_ENGINE_SKIP = 10 ** 9

ENGS = ("pe", "act", "dve", "pool", "sp")


class Buf:
    __slots__ = ("name", "writers", "readers", "dsem", "dcount")

    def __init__(self, name):
        self.name = name
        self.writers = {}
        self.readers = {}
        self.dsem = None
        self.dcount = 0


class Prog:
    def __init__(self, nc):
        self.nc = nc
        self.ops = {e: [] for e in ENGS}
        self.nsem = 0
        self.dma_bufs = []
        self._dsem_of = {}
        self._keep = []
        self.defer = None

    def play(self, item):
        kind, eng, fn, reads, writes = item
        if kind == "op":
            self.op(eng, fn, list(reads), list(writes))
        else:
            self.dma(eng, fn, list(reads), list(writes))

    def _new_dma_sem(self):
        cm = self.nc.semaphore("dq%d" % self.nsem)
        self.nsem += 1
        h = cm.__enter__()
        self._keep.append(cm)
        return h

    def _collect(self, eng, reads, writes):
        deps = {}

        here = len(self.ops[eng])

        def add(k, v):
            if k == eng and eng == "pe":
                return
            if k == eng and eng in ("dve", "act") and here - v >= SAME_ENGINE_SKIP:
                return
            if deps.get(k, -1) < v:
                deps[k] = v
        for b in reads:
            for k, v in b.writers.items():
                add(k, v)
        for b in writes:
            for k, v in b.readers.items():
                add(k, v)
            for k, v in b.writers.items():
                add(k, v)
        return deps

    def _update(self, key, val, reads, writes):
        for b in reads:
            if b.readers.get(key, -1) < val:
                b.readers[key] = val
        for b in writes:
            if b.readers:
                b.readers = {}
                b.writers = {}
            if b.writers.get(key, -1) < val:
                b.writers[key] = val

    def op(self, eng, fn, reads=(), writes=()):
        if self.defer is not None:
            self.defer.append(("op", eng, fn, tuple(reads), tuple(writes)))
            return
        deps = self._collect(eng, reads, writes)
        pos = len(self.ops[eng])
        self.ops[eng].append({"deps": deps, "fn": fn, "dma": None, "need_inc": False})
        self._update(eng, pos, reads, writes)

    def dma(self, eng, fn, reads=(), writes=(), n=1):
        if self.defer is not None:
            self.defer.append(("dma", eng, fn, tuple(reads), tuple(writes)))
            return
        deps = self._collect(eng, reads, writes)
        owner = writes[0] if writes else reads[0]
        cls = "sw" if eng == "pool" else "hw"
        if owner.dsem is None:
            owner.dsem = {}
        if cls not in owner.dsem:
            owner.dsem[cls] = [self._new_dma_sem(), 0]
            self.dma_bufs.append(owner.dsem[cls])
        ent = owner.dsem[cls]
        ent[1] += 16 * n
        key = ("dma", id(owner), cls)
        self._dsem_of[key] = ent[0]
        self.ops[eng].append({"deps": deps, "fn": fn, "dma": ent[0], "need_inc": False})
        self._update(key, ent[1], reads, writes)

    def barrier(self):
        last = {}
        for e in ENGS:
            idx = [i for i, o in enumerate(self.ops[e]) if o["fn"] is not None and o["dma"] is None]
            if idx:
                last[e] = idx[-1]
        for e in ENGS:
            deps = {k: v for k, v in last.items() if k != e}
            self.ops[e].append({"deps": deps, "fn": None, "dma": None, "need_inc": False})

    def emit(self):
        nc = self.nc
        for e in ENGS:
            for o in self.ops[e]:
                for k, v in o["deps"].items():
                    if isinstance(k, str):
                        self.ops[k][v]["need_inc"] = True
        cnt_at = {}
        for e in ENGS:
            c = 0
            arr = []
            for o in self.ops[e]:
                if o["need_inc"]:
                    assert o["fn"] is not None and o["dma"] is None
                    c += 1
                arr.append(c)
            cnt_at[e] = arr
        sems = {}
        for e in ENGS:
            if e == "sp":
                continue
            cm = nc.semaphore("c_" + e)
            sems[e] = cm.__enter__()
            self._keep.append(cm)
        handles = {"pe": "tensor", "act": "scalar", "dve": "vector", "pool": "gpsimd", "sp": "sync"}
        with nc.Block() as block:
            for e in ENGS:
                ops = self.ops[e]

                def body(eh, e=e, ops=ops):
                    seen = {}
                    for o in ops:
                        for k, v in o["deps"].items():
                            if isinstance(k, str):
                                sem = sems[k]
                                val = cnt_at[k][v]
                            else:
                                sem = self._dsem_of[k]
                                val = v
                            if seen.get(sem.num, -1) >= val:
                                continue
                            seen[sem.num] = val
                            eh.wait_ge(sem, val)
                        if o["fn"] is None:
                            continue
                        ins = o["fn"](eh)
                        if o["dma"] is not None:
                            ins.then_inc(o["dma"], 16)
                        elif o["need_inc"]:
                            ins.then_inc(sems[e], 1)
                    if e == "sp":
                        for ent in self.dma_bufs:
                            eh.wait_ge(ent[0], ent[1])
                getattr(block, handles[e])(body)


def host_consts():
    s = np.arange(128)
    same = (s[:, None] // 64) == (s[None, :] // 64)
    c = {}
    c["c_ident"] = np.eye(128, dtype=np.float32)
    c["c_btri"] = (same & (s[:, None] <= s[None, :])).astype(np.float32)
    c["c_negtri"] = -(s[:, None] >= s[None, :]).astype(np.float32)
    c["c_stri"] = (s[:, None] < s[None, :]).astype(np.float32)
    c["c_bones"] = same.astype(np.float32)
    c["c_ones"] = np.ones((128, 128), np.float32)
    ci = np.zeros((128, 2), np.float32)
    ci[:64, 0] = 1.0
    ci[64:, 1] = 1.0
    c["c_cind"] = ci
    return c


def build(nc, dbg=False, maxq=NSB, stop=None, nexp=NE):
    P = Prog(nc)

    def dram_in(name, shape):
        return nc.dram_tensor(name, list(shape), F32, kind="ExternalInput").ap()

    x_d = dram_in("x", [SEQ, D])
    meta_d = dram_in("meta_tokens", [NMETA, D])
    w_in_d = dram_in("w_in", [D, 3584])
    lbp_d = dram_in("hg_lower_bound", [2, 512])
    hgg_d = dram_in("hg_norm_g", [1, 512])
    sbg_d = dram_in("sb_norm_g", [1, 512])
    w_out_d = dram_in("w_out", [D, D])
    ln1g_d = dram_in("ln1_g", [1, D])
    ln1b_d = dram_in("ln1_b", [1, D])
    wrg_d = dram_in("w_router_group", [D, 4])
    brg_d = dram_in("b_router_group", [1, 4])
    wre_d = dram_in("w_router_expert", [D, 16])
    bre_d = dram_in("b_router_expert", [1, 16])
    w1_d = dram_in("w_exp_gate", [NE, D, 512])
    w3_d = dram_in("w_exp_up", [NE, D, 512])
    w2_d = dram_in("w_exp_down", [NE, 512, D])
    ln2g_d = dram_in("ln2_g", [1, D])
    ln2b_d = dram_in("ln2_b", [1, D])
    cst = {k: dram_in(k, v.shape) for k, v in host_consts().items()}
    out_d = nc.dram_tensor("out", [SEQ, D], F32, kind="ExternalOutput").ap()
    if dbg:
        dbg_mixT = nc.dram_tensor("dbg_mixT", [8, 128, LP], BF16, kind="ExternalOutput").ap()
        dbg_h1 = nc.dram_tensor("dbg_h1", [LP, D], F32, kind="ExternalOutput").ap()

    w_in_bf = nc.dram_tensor("w_in_bf", [D, 3584], BF16).ap()
    w_out_bf = nc.dram_tensor("w_out_bf", [D, D], BF16).ap()
    w1_bf = nc.dram_tensor("w1_bf", [NE, D, 512], BF16).ap()
    w3_bf = nc.dram_tensor("w3_bf", [NE, D, 512], BF16).ap()
    w2_bf = nc.dram_tensor("w2_bf", [NE, 512, D], BF16).ap()

    sb_lo = (nc.sbuf_base + 63) // 64 * 64
    sb_hi = nc.sbuf_top
    cur = [sb_lo]
    cnt = [0]

    def sb(shape, dt):
        n = 1
        for s_ in shape[1:]:
            n *= s_
        nbytes = n * (4 if dt == F32 else 2)
        nbytes = (nbytes + 63) // 64 * 64
        off = cur[0]
        cur[0] += nbytes
        assert cur[0] <= sb_hi, ("SBUF overflow", cur[0], sb_hi)
        cnt[0] += 1
        return nc.alloc_sbuf_tensor_at("t%d" % cnt[0], list(shape), dt, offset=off)

    psf = [nc.alloc_psum_tensor("psf%d" % i, [128, 512], F32) for i in range(8)]
    nc.psum_base = 0
    psb = [nc.alloc_psum_tensor("psb%d" % i, [128, 1024], BF16) for i in range(8)]
    PB = [Buf("bank%d" % i) for i in range(8)]

    kT = sb([128, 4, LP], BF16)
    vv = sb([128, NT, 512], BF16)
    BkT = [Buf("kT%d" % i) for i in range(NSB)]
    Bv = [Buf("v%d" % i) for i in range(NSB)]
    ident_b = sb([128, 128], BF16)
    ident_f = sb([128, 128], F32)
    btri_f = sb([128, 128], F32)
    btri_b = sb([128, 128], BF16)
    negtri_b = sb([128, 128], BF16)
    stri_b = sb([128, 128], BF16)
    bones_b = sb([128, 128], BF16)
    ones_b = sb([128, 128], BF16)
    ones_f = sb([128, 128], F32)
    cind_f = sb([128, 2], F32)
    ln1g = sb([128, D], F32)
    ln1b = sb([128, D], F32)
    ln2g = sb([128, D], F32)
    ln2b = sb([128, D], F32)
    lb_rep = sb([128, 512], F32)
    oml_rep = sb([128, 512], F32)
    hgg_rep = sb([128, 512], F32)
    sbgT = sb([128, 4], F32)
    w_r = sb([128, 8, 20], F32)
    b_r = sb([1, 20], F32)
    S_f = sb([128, 4, 128], F32)
    S_b = sb([128, 4, 128], BF16)
    tmpS = [sb([128, 128], F32) for _ in range(2)]
    h1 = sb([128, SBT, D], F32)
    TT = sb([128, 8, 512], F32)
    st6 = sb([128, SBT, 12], F32)
    mv = sb([128, SBT, 2], F32)
    lnr = sb([128, SBT, 2], F32)
    Bln = [Buf("ln%d" % j) for j in range(SBT)]
    Bconst = Buf("const")
    BS = [Buf("S%d" % h) for h in range(4)]
    BtmpS = [Buf("tmpS0"), Buf("tmpS1")]
    Bh1 = [Buf("h1_%d" % j) for j in range(SBT)]
    BT = [Buf("T%d" % i) for i in range(8)]

    region_lo = cur[0]

    xb = sb([128, D], BF16)
    hT = sb([128, 8, W], BF16)
    wblk = [sb([128, 8, 512], BF16) for _ in range(2)]
    hq_sb = sb([128, SBT, 512], BF16)
    hf_sb = sb([128, SBT, 512], F32)
    hi_sb = sb([128, SBT, 512], BF16)
    hg_sb = sb([128, SBT, 512], BF16)
    qT = sb([128, 4, W], BF16)
    mixT = sb([128, 8, W], BF16)
    qtl = sb([128, 512], BF16)
    ktl = sb([128, 512], BF16)
    qkT = sb([128, 8, 128], BF16)
    At = sb([128, 4, 128], BF16)
    mx = sb([128, 512], BF16)
    el = sb([128, 8], F32)
    ssq = sb([128, 4], F32)
    rstd4 = sb([128, 4], F32)
    Lp = [[sb([128, W], BF16) for _ in range(2)] for _ in range(2)]
    Aa = [[sb([128, W], BF16) for _ in range(2)] for _ in range(2)]
    r_sb = [sb([1, W], BF16) for _ in range(4)]
    Ef = [[sb([128, W], F32) for _ in range(3)] for _ in range(2)]
    Osb_t = sb([128, W], F32)
    rs_t = sb([128, W], F32)
    Xf = [sb([128, W], F32) for _ in range(2)]
    sqb = sb([128, W], BF16)
    Bxb = Buf("xb"); BhT = Buf("hT"); Bwblk = [Buf("wblk0"), Buf("wblk1")]
    Bhq = [Buf("hq%d" % j) for j in range(SBT)]
    Bhf = [Buf("hf%d" % j) for j in range(SBT)]
    Bhi = [Buf("hi%d" % j) for j in range(SBT)]
    Bhg = [Buf("hg%d" % j) for j in range(SBT)]
    BqT = Buf("qT")
    BmixT = [Buf("mixT%d" % k) for k in range(8)]
    Bqtl = Buf("qtl"); Bktl = Buf("ktl"); BqkT = Buf("qkT"); BAt = Buf("At"); Bmx = Buf("mx")
    Bel = Buf("el"); Bssq = Buf("ssq"); Brstd4 = Buf("rstd4")
    BLp = [[Buf("Lp%d%d" % (a_, b_)) for b_ in range(2)] for a_ in range(2)]; BAa = [[Buf("A%d%d" % (a_, b_)) for b_ in range(2)] for a_ in range(2)]; Br = [Buf("r%d" % i) for i in range(4)]; BEf = [[Buf("Ef%d%d" % (a_, b_)) for b_ in range(3)] for a_ in range(2)]; BXf = [Buf("Xf0"), Buf("Xf1")]; BOsb = Buf("Osb"); Brs = Buf("rs")
    Bsqb = Buf("sqb")
    p1_hi = cur[0]

    cur[0] = region_lo
    h1T_f = sb([128, 8, 128], F32)
    h1T_b = sb([128, 8, W], BF16)
    wslot = [sb([128, 8, 512], BF16) for _ in range(7)]
    lg = sb([128, SBT, 20], F32)
    rt = sb([128, SBT, 64], F32)
    lem = sb([128, SBT, 16], F32)
    top8 = sb([128, SBT, 8], F32)
    m1 = sb([128, SBT, 16], F32)
    m2 = sb([128, SBT, 16], F32)
    gates = sb([128, SBT, 16], F32)
    De = [sb([128, 128], BF16) for _ in range(2)]
    Gb = [sb([128, W], BF16) for _ in range(2)]
    s_sb = [sb([128, W], BF16) for _ in range(2)]
    u_sb = [sb([128, W], BF16) for _ in range(2)]
    hidg = [sb([128, 4, W], BF16) for _ in range(2)]
    Bh1Tf = Buf("h1Tf"); Bh1Tb = [Buf("h1Tb%d" % j) for j in range(SBT)]
    Bws = [Buf("ws%d" % i) for i in range(7)]
    Blg = [Buf("lg%d" % j) for j in range(SBT)]; Brt = [Buf("rt%d" % j) for j in range(SBT)]; Bgates = [Buf("gates%d" % j) for j in range(SBT)]
    BDe = [Buf("De0"), Buf("De1")]; BGb = [Buf("Gb0"), Buf("Gb1")]
    Bs = [Buf("s0"), Buf("s1")]; Bu = [Buf("u0"), Buf("u1")]; Bhidg = [Buf("hidg0"), Buf("hidg1")]
    p2_hi = cur[0]
    cur[0] = max(p1_hi, p2_hi)

    def mm(out, lhsT, rhs, start, stop, reads, writes):
        P.op("pe", lambda e: e.matmul(out, lhsT=lhsT, rhs=rhs, start=start, stop=stop, skip_group_check=True),
             reads, writes)

    def tr(out, in_, ident, reads, writes):
        P.op("pe", lambda e: e.transpose(out=out, in_=in_, identity=ident), reads, writes)

    def act(out, in_, func, reads, writes, **kw):
        P.op("act", lambda e: e.activation(out=out, in_=in_, func=func, **kw), reads, writes)

    def tt(eng, out, in0, in1, op, reads, writes):
        P.op(eng, lambda e: e.tensor_tensor(out=out, in0=in0, in1=in1, op=op), reads, writes)

    def ts(eng, out, in0, s1, op0, reads, writes, s2=None, op1=None):
        if op1 is None:
            P.op(eng, lambda e: e.tensor_scalar(out=out, in0=in0, scalar1=s1, scalar2=None, op0=op0), reads, writes)
        else:
            P.op(eng, lambda e: e.tensor_scalar(out=out, in0=in0, scalar1=s1, scalar2=s2, op0=op0, op1=op1),
                 reads, writes)

    def stt(out, in0, scalar, in1, op0, op1, reads, writes):
        P.op("dve", lambda e: e.scalar_tensor_tensor(out=out, in0=in0, scalar=scalar, in1=in1, op0=op0, op1=op1),
             reads, writes)

    def recip(out, in_, reads, writes):
        P.op("dve", lambda e: e.reciprocal(out=out, in_=in_), reads, writes)

    flip = [0]

    def evac(out, in_, reads, writes, scale=None):
        flip[0] ^= 1
        if flip[0]:
            if scale is None:
                act(out, in_, AF.Copy, reads, writes)
            else:
                act(out, in_, AF.Copy, reads, writes, scale=scale)
        else:
            if scale is None:
                P.op("dve", lambda e: e.tensor_copy(out=out, in_=in_), reads, writes)
            else:
                ts("dve", out, in_, scale, ALU.mult, reads, writes)

    def bcast_rows(src_ap_1xn, n):
        return bass.AP(tensor=src_ap_1xn.tensor, offset=src_ap_1xn.offset, ap=[[0, 128], [1, n]])

    Bwin = Buf("w_in_bf"); Bwout = Buf("w_out_bf"); Bexp = [Buf("exp%d" % e) for e in range(NE)]
    for r in range(8):
        P.dma("pool", lambda e, r=r: e.dma_start(out=w_in_bf[r * 128:(r + 1) * 128, :], in_=w_in_d[r * 128:(r + 1) * 128, :]),
              writes=[Bwin])
    for r in range(4):
        P.dma("pool", lambda e, r=r: e.dma_start(out=w_out_bf[r * 256:(r + 1) * 256, :], in_=w_out_d[r * 256:(r + 1) * 256, :]),
              writes=[Bwout])
    for nm, ap in (("c_ident", ident_b), ("c_btri", btri_b), ("c_negtri", negtri_b), ("c_stri", stri_b),
                   ("c_bones", bones_b), ("c_ones", ones_b)):
        P.dma("pool", lambda e, nm=nm, ap=ap: e.dma_start(out=ap[:], in_=cst[nm]), writes=[Bconst])
    for nm, ap in (("c_ident", ident_f), ("c_btri", btri_f), ("c_ones", ones_f), ("c_cind", cind_f)):
        P.dma("sp", lambda e, nm=nm, ap=ap: e.dma_start(out=ap[:], in_=cst[nm]), writes=[Bconst])
    for src, dst, n in ((ln1g_d, ln1g, D), (ln1b_d, ln1b, D), (ln2g_d, ln2g, D), (ln2b_d, ln2b, D),
                        (hgg_d, hgg_rep, 512), (lbp_d[0:1, :], lb_rep, 512), (lbp_d[1:2, :], oml_rep, 512)):
        P.dma("sp", lambda e, src=src, dst=dst, n=n: e.dma_start(out=dst[:], in_=bcast_rows(src, n)), writes=[Bconst])
    P.dma("sp", lambda e: e.dma_start(out=sbgT[:], in_=sbg_d.rearrange("o (p q) -> q (o p)", q=128), allow_slow_non_contiguous=True), writes=[Bconst])
    P.dma("sp", lambda e: e.dma_start(out=w_r[:, :, 0:4], in_=wrg_d.rearrange("(k p) g -> p k g", p=128), allow_slow_non_contiguous=True), writes=[Bconst])
    P.dma("sp", lambda e: e.dma_start(out=w_r[:, :, 4:20], in_=wre_d.rearrange("(k p) g -> p k g", p=128), allow_slow_non_contiguous=True), writes=[Bconst])
    P.dma("sp", lambda e: e.dma_start(out=b_r[:, 0:4], in_=brg_d), writes=[Bconst])
    P.dma("sp", lambda e: e.dma_start(out=b_r[:, 4:20], in_=bre_d), writes=[Bconst])
    cast_next = [0]

    def issue_cast(n=1):
        for _ in range(n):
            e_ = cast_next[0]
            if e_ >= nexp:
                return
            cast_next[0] += 1
            thr = [Bexp[e_ - 2]] if e_ >= 2 else [Bwout, Bconst]
            for (src, dst) in ((w1_d, w1_bf), (w3_d, w3_bf)):
                for hh in range(2):
                    P.dma("pool", lambda e, src=src, dst=dst, e_=e_, hh=hh: e.dma_start(
                        out=dst[e_, hh * 512:(hh + 1) * 512, :], in_=src[e_, hh * 512:(hh + 1) * 512, :]),
                        reads=thr, writes=[Bexp[e_]])
            for hh in range(2):
                P.dma("pool", lambda e, e_=e_, hh=hh: e.dma_start(
                    out=w2_bf[e_, hh * 256:(hh + 1) * 256, :], in_=w2_d[e_, hh * 256:(hh + 1) * 256, :]),
                    reads=thr, writes=[Bexp[e_]])

    issue_cast(NE)
    tt("dve", oml_rep[:], oml_rep[:], lb_rep[:], ALU.subtract, [Bconst], [Bconst])
    act(oml_rep[:], oml_rep[:], AF.Exp, [Bconst], [Bconst])
    ts("dve", lb_rep[:], oml_rep[:], 1.0, ALU.add, [Bconst], [Bconst])
    recip(lb_rep[:], lb_rep[:], [Bconst], [Bconst])
    tt("dve", oml_rep[:], oml_rep[:], lb_rep[:], ALU.mult, [Bconst], [Bconst])
    P.op("dve", lambda e: e.memset(S_f[:], 0.0), [], BS)
    P.op("dve", lambda e: e.memset(S_b[:], 0.0), [], BS)

    def Tf(i, n=512):
        return TT[:, i, 0:n]

    def load_x_rows(eng, dst, i, Bdst):
        if i == 0:
            P.dma(eng, lambda e: e.dma_start(out=dst[0:16, :], in_=meta_d[:, :]), writes=[Bdst])
            P.dma(eng, lambda e: e.dma_start(out=dst[16:128, :], in_=x_d[0:112, :]), writes=[Bdst])
        elif i < NT - 1:
            P.dma(eng, lambda e: e.dma_start(out=dst[:, :], in_=x_d[128 * i - 16:128 * i + 112, :]), writes=[Bdst])
        else:
            P.op("dve", lambda e: e.memset(dst[:, :], 0.0), [], [Bdst])
            P.dma(eng, lambda e: e.dma_start(out=dst[0:16, :], in_=x_d[SEQ - 16:SEQ, :]), writes=[Bdst])

    wctr = [0]

    def load_wblk(src3):
        i = wctr[0] % 2
        wctr[0] += 1
        P.dma("sp", lambda e: e.dma_start(out=wblk[i][:], in_=src3), reads=[Bwin, Bwout], writes=[Bwblk[i]])
        return i

    bankrr = [0]

    def next_bank(lo=0, hi=6):
        b = lo + bankrr[0] % (hi - lo)
        bankrr[0] += 1
        return b

    pending_ln2 = [[]]

    def flush_ln2(n):
        lst = pending_ln2[0]
        for _ in range(min(n, len(lst))):
            P.play(lst.pop(0))

    for Q in range(min(NSB, maxq)):
        t0 = Q * SBT
        PL = "dve" if Q == 0 else "pool"
        tok0 = t0 * 128
        for j in range(SBT):
            xst = TT[:, 6:8, :].rearrange("p a b -> p (a b)")
            load_x_rows("sp", xst, t0 + j, BT[6])
            act(xb[:, :], xst, AF.Copy, [BT[6]], [Bxb])
            for k in range(8):
                tr(psb[7][:, k * 128:(k + 1) * 128], xb[:, k * 128:(k + 1) * 128], ident_b[:], [Bxb, Bconst], [PB[7]])
            evac(hT[:, :, j * 128:(j + 1) * 128], psb[7][:, :].rearrange("p (k t) -> p k t", k=8), [PB[7]], [BhT])
        for cb in (1, 0, 2, 3, 6, 4, 5):
            wi = load_wblk(w_in_bf[:, cb * 512:(cb + 1) * 512].rearrange("(k p) c -> p k c", p=128))
            if cb in (4, 5):
                for p in range(4):
                    b = next_bank()
                    for k in range(8):
                        mm(psf[b][:, 0:W], wblk[wi][:, k, p * 128:(p + 1) * 128], hT[:, k, :], k == 0, k == 7,
                           [Bwblk[wi], BhT], [PB[b]])
                    if cb == 4:
                        evac(qT[:, p, :], psf[b][:, 0:W], [PB[b]], [BqT], scale=0.125)
                    else:
                        evac(kT[:, p, tok0:tok0 + W], psf[b][:, 0:W], [PB[b]], [BkT[Q]])
                    flush_ln2(2)
            else:
                for j in range(SBT):
                    b = next_bank()
                    for k in range(8):
                        mm(psf[b][:, :], hT[:, k, j * 128:(j + 1) * 128], wblk[wi][:, k, :], k == 0, k == 7,
                           [Bwblk[wi], BhT], [PB[b]])
                    if cb == 0:
                        evac(hq_sb[:, j, :], psf[b][:, :], [PB[b]], [Bhq[j]])
                    elif cb == 1:
                        evac(hf_sb[:, j, :], psf[b][:, :], [PB[b]], [Bhf[j]])
                    elif cb == 2:
                        evac(hi_sb[:, j, :], psf[b][:, :], [PB[b]], [Bhi[j]])
                    elif cb == 3:
                        evac(hg_sb[:, j, :], psf[b][:, :], [PB[b]], [Bhg[j]])
                    else:
                        evac(vv[:, t0 + j, :], psf[b][:, :], [PB[b]], [Bv[Q]])
                    flush_ln2(2)

        flush_ln2(10 ** 6)
        if stop == '1a':
            continue
        deferred = []
        P.defer = deferred
        for j in range(SBT):
            t1, t2, t3, t4, t5, t6, t7 = (Tf(i) for i in range(7))
            B1, B2, B3, B4, B5, B6, B7 = BT[0:7]
            if Q == 0:
                issue_cast(1)
            act(t1, hf_sb[:, j, :], AF.Sigmoid, [Bhf[j]], [B1])
            deferred.append("GLUE")
            act(t6, hq_sb[:, j, :], AF.Sigmoid, [Bhq[j]], [B6])
            deferred.append("GLUE")
            act(t7, hg_sb[:, j, :], AF.Sigmoid, [Bhg[j]], [B7])
            tt("dve", t1, t1, oml_rep[:], ALU.mult, [B1, Bconst], [B1])
            tt("dve", t6, t6, hq_sb[:, j, :], ALU.mult, [B6, Bhq[j]], [B6])
            tt("dve", t7, t7, hgg_rep[:], ALU.mult, [B7, Bconst], [B7])
            tt("dve", t1, t1, lb_rep[:], ALU.add, [B1, Bconst], [B1])
            act(t2, t1, AF.Ln, [B1], [B2])
            ts("dve", t3, t1, -1.0, ALU.mult, [B1], [B3], s2=1.0, op1=ALU.add)
            o_sb = Tf(7)
            for h in range(4):
                mm(psf[7][:, 2 * h:2 * h + 2], t2[:, h * 128:(h + 1) * 128], cind_f[:], h == 0, h == 3,
                   [Bconst, B2], [PB[7]])
            act(el[:], psf[7][:, 0:8], AF.Exp, [PB[7]], [Bel])
            deferred.append(None)
            mm(psf[7][:, :], btri_f[:], t2, True, True, [Bconst, B2], [PB[7]])
            act(t4, psf[7][:, :], AF.Exp, [PB[7]], [B4])
            act(t5, psf[7][:, :], AF.Exp, [PB[7]], [B5], scale=-1.0)
            deferred.append(None)
            tt("dve", qtl[:], t6, t4, ALU.mult, [B6, B4], [Bqtl])
            tt("dve", ktl[:], t3, t5, ALU.mult, [B3, B5], [Bktl])
            for h in range(4):
                tr(psb[7][:, h * 128:(h + 1) * 128], qtl[:, h * 128:(h + 1) * 128], ident_b[:], [Bqtl, Bconst], [PB[7]])
            for h in range(4):
                tr(psb[7][:, (4 + h) * 128:(5 + h) * 128], ktl[:, h * 128:(h + 1) * 128], ident_b[:], [Bktl, Bconst], [PB[7]])
            evac(qkT[:, :, :], psb[7][:, :].rearrange("p (k t) -> p k t", k=8), [PB[7]], [BqkT])
            deferred.append(None)
            for h in range(4):
                mm(psf[7][:, h * 128:(h + 1) * 128], qkT[:, 4 + h, :], qkT[:, h, :], h == 0, h == 3, [BqkT], [PB[7]])
            btri_bc = bass.AP(tensor=btri_b, offset=0, ap=[[128, 128], [0, 4], [1, 128]])
            tt("dve", At[:, :, :], psf[7][:, :].rearrange("p (h t) -> p h t", h=4), btri_bc, ALU.mult,
               [PB[7], Bconst], [BAt])
            deferred.append(None)
            for h in range(4):
                hs = slice(h * 128, (h + 1) * 128)
                mm(psf[7][:, hs], At[:, h, :], hi_sb[:, j, hs], h == 0, h == 3, [BAt, Bhi[j]], [PB[7]])
            evac(o_sb, psf[7][:, :], [PB[7]], [BT[7]])
            deferred.append(None)
            for c in range(2):
                rows = slice(64 * c, 64 * c + 64)
                for h in range(4):
                    hs = slice(h * 128, (h + 1) * 128)
                    mm(psf[7][rows, hs], qkT[:, h, rows], S_b[:, h, :], h == 0, h == 3, [BqkT, BS[h]], [PB[7]])
                tt("dve", o_sb[rows, :], psf[7][rows, :], o_sb[rows, :], ALU.add, [PB[7], BT[7]], [BT[7]])
                deferred.append(None)
                for h in range(4):
                    hs = slice(h * 128, (h + 1) * 128)
                    mm(psf[7][:, hs], ktl[rows, hs], hi_sb[rows, j, hs], h == 0, h == 3, [Bktl, Bhi[j]], [PB[7]])
                S_f2 = S_f[:, :, :].rearrange("p h d -> p (h d)")
                tt("dve", t2, psf[7][:, :], S_f2, ALU.add, [PB[7]] + BS, [B2])
                deferred.append(None)
                el_bc = bass.AP(tensor=el, offset=c, ap=[[8, 128], [2, 4], [0, 128]])
                t2v = t2.rearrange("p (h d) -> p h d", h=4)
                tt("dve", S_b[:, :, :], t2v, el_bc, ALU.mult, [B2, Bel], BS)
                tt("dve", S_f[:, :, :], t2v, el_bc, ALU.mult, [B2, Bel], BS)
            P.op(PL, lambda e: e.memset(ssq[:], 0.0), [], [Bssq])
            for h in range(4):
                hs = slice(h * 128, (h + 1) * 128)
                act(t1[:, hs], o_sb[:, hs], AF.Square, [BT[7]], [B1, Bssq], accum_out=ssq[:, h:h + 1])
            ts("dve", rstd4[:], ssq[:], 1.0 / 128.0, ALU.mult, [Bssq], [Brstd4], s2=RMS_EPS, op1=ALU.add)
            act(rstd4[:], rstd4[:], AF.Ln, [Brstd4], [Brstd4])
            act(rstd4[:], rstd4[:], AF.Exp, [Brstd4], [Brstd4], scale=-0.5)
            for h in range(4):
                hs = slice(h * 128, (h + 1) * 128)
                stt(mx[:, hs], o_sb[:, hs], rstd4[:, h:h + 1], t7[:, hs], ALU.mult, ALU.mult,
                    [BT[7], Brstd4, B7], [Bmx])
            for h in range(4):
                tr(psb[7][:, h * 128:(h + 1) * 128], mx[:, h * 128:(h + 1) * 128], ident_b[:], [Bmx, Bconst], [PB[7]])
            evac(mixT[:, 0:4, j * 128:(j + 1) * 128], psb[7][:, 0:512].rearrange("p (k t) -> p k t", k=4),
                 [PB[7]], BmixT[0:4])
            deferred.append(None)

        if stop == '1b':
            for it_ in deferred:
                if it_ is not None and it_ != "GLUE":
                    P.play(it_)
            continue
        P.defer = None
        jmax = t0 + SBT - 1
        groups = [(p, j) for p in range(4) for j in range(jmax, -1, -1)]
        ng = len(groups)
        PRS = (slice(0, 64), slice(64, 128))

        def ginfo(g):
            p, j = groups[g]
            c0 = 128 * max(0, j - t0)
            return dict(p=p, j=j, c0=c0, cs=slice(c0, W), first=(j == jmax), Qi=j // SBT, ob=4,
                        e3=g % 3, s2=g % 2, ri=[(p % 2) * 2, (p % 2) * 2 + 1],
                        cb=[2, 3] if g % 2 == 0 else [5, 6])

        def sA(g):
            t = ginfo(g)
            cs = t["cs"]
            if t["first"]:
                for hf_ in range(2):
                    P.op(PL, lambda e, ri=t["ri"][hf_]: e.memset(r_sb[ri][:], 0.0), [], [Br[t["ri"][hf_]]])
            for hf_ in range(2):
                mm(psf[hf_][:, cs], kT[PRS[hf_], t["p"], t["j"] * 128:(t["j"] + 1) * 128], qT[PRS[hf_], t["p"], cs], True, True,
                   [BkT[t["Qi"]], BqT], [PB[0], PB[1]] if hf_ == 0 else [PB[1]])

        def sB(g):
            t = ginfo(g)
            cs, c0, e3, s2 = t["cs"], t["c0"], t["e3"], t["s2"]
            for hf_ in range(2):
                act(Ef[hf_][e3][:, cs], psf[hf_][:, cs], AF.Exp, [PB[hf_]], [BEf[hf_][e3]])
            for hf_ in range(2):
                act(Lp[hf_][s2][:, cs], Ef[hf_][e3][:, cs], AF.Ln, [BEf[hf_][e3]], [BLp[hf_][s2]], bias=1.0)
            if t["j"] >= t0:
                for hf_ in range(2):
                    tt("dve", Lp[hf_][s2][:, c0:c0 + 128], Lp[hf_][s2][:, c0:c0 + 128], stri_b[:], ALU.mult,
                       [BLp[hf_][s2], Bconst], [BLp[hf_][s2]])

        def sC(g):
            t = ginfo(g)
            cs, s2, cb = t["cs"], t["s2"], t["cb"]
            for hf_ in range(2):
                mm(psf[cb[hf_]][:, cs], negtri_b[:], Lp[hf_][s2][:, cs], True, t["first"],
                   [Bconst, BLp[0][s2], BLp[1][s2]] if hf_ == 0 else [Bconst, BLp[1][s2]],
                   [PB[cb[0]], PB[cb[1]]] if hf_ == 0 else [PB[cb[1]]])
            if not t["first"]:
                for hf_ in range(2):
                    ri = t["ri"][hf_]
                    mm(psf[cb[hf_]][:, cs], ones_b[0:1, :], r_sb[ri][0:1, cs], False, True,
                       [Bconst, Br[t["ri"][0]], Br[t["ri"][1]]] if hf_ == 0 else [Bconst, Br[ri]], [PB[cb[hf_]]])

        def sD(g):
            t = ginfo(g)
            cs, c0, e3, s2, cb = t["cs"], t["c0"], t["e3"], t["s2"], t["cb"]
            for hf_ in range(2):
                ri = t["ri"][hf_]
                if t["j"] > 0:
                    P.op("dve", lambda e, ri=ri, hf_=hf_: e.tensor_copy(out=r_sb[ri][0:1, cs], in_=psf[cb[hf_]][0:1, cs]),
                         [PB[cb[hf_]]], [Br[ri]])
                act(Xf[hf_][:, cs], psf[cb[hf_]][:, cs], AF.Exp, [PB[cb[hf_]], Br[ri]], [BXf[hf_]])
            for hf_ in range(2):
                eng_ = "dve" if hf_ == 0 else PL
                tt(eng_, Aa[hf_][s2][:, cs], Ef[hf_][e3][:, cs], Xf[hf_][:, cs], ALU.mult, [BEf[hf_][e3], BXf[hf_]], [BAa[hf_][s2]])
                if t["j"] >= t0:
                    tt(eng_, Aa[hf_][s2][:, c0:c0 + 128], Aa[hf_][s2][:, c0:c0 + 128], stri_b[:], ALU.mult,
                       [BAa[hf_][s2], Bconst], [BAa[hf_][s2]])

        def sF(g):
            t = ginfo(g)
            cs, ob, p, s2 = t["cs"], t["ob"], t["p"], t["s2"]
            for hf_ in range(2):
                h = 2 * p + hf_
                mm(psf[ob][PRS[hf_], cs], vv[:, t["j"], h * 64:(h + 1) * 64], Aa[hf_][s2][:, cs], t["first"], t["j"] == 0,
                   [Bv[t["Qi"]], BAa[0][s2], BAa[1][s2]] if hf_ == 0 else [Bv[t["Qi"]], BAa[1][s2]], [PB[ob]])
            if t["j"] == 0:
                Osb = Osb_t[:, :]
                rs = rs_t[:, :]
                act(Osb, psf[ob][:, 0:W], AF.Copy, [PB[ob]], [BOsb])
                act(sqb[:], psf[ob][:, 0:W], AF.Square, [PB[ob]], [Bsqb])
                mm(psf[7][:, 0:W], bones_b[:], sqb[:], True, True, [Bconst, Bsqb], [PB[7]])
                ts("dve", rs, psf[7][:, 0:W], 1.0 / 64.0, ALU.mult, [PB[7]], [Brs], s2=RMS_EPS, op1=ALU.add)
                act(rs, rs, AF.Ln, [Brs], [Brs])
                act(rs, rs, AF.Exp, [Brs], [Brs], scale=-0.5)
                stt(mixT[:, 4 + p, :], Osb, sbgT[:, p:p + 1], rs, ALU.mult, ALU.mult, [BOsb, Brs, Bconst], [BmixT[4 + p]])

        stages = ((sF, 4), (sD, 3), (sC, 2), (sB, 1), (sA, 0))
        nit = ng + 4
        ndef = sum(1 for x in deferred if x is not None and x != "GLUE")
        per_it = -(-ndef // nit)
        in_b7 = [False] * len(deferred)
        open_ = False
        for i_, item in enumerate(deferred):
            if item is None:
                open_ = False
                continue
            if item == "GLUE":
                continue
            in_b7[i_] = open_
            if PB[7] in item[4]:
                open_ = True
        dpos = 0
        snaps = []

        def ready(item, it):
            if it < 1:
                return True
            deps = P._collect(item[1], list(item[3]), list(item[4]))
            lim = snaps[it - 1]
            for k, v in deps.items():
                if isinstance(k, str) and v >= lim[k]:
                    return False
            return True

        for it in range(nit):
            snaps.append({e: len(P.ops[e]) for e in ENGS})
            for fn_, lag in stages:
                g = it - lag
                if 0 <= g < ng:
                    fn_(g)
            played = 0
            glue = False
            forced = False
            while dpos < len(deferred):
                item = deferred[dpos]
                if item is None:
                    dpos += 1
                    forced = False
                    continue
                if item == "GLUE":
                    dpos += 1
                    glue = True
                    continue
                must = glue or in_b7[dpos]
                if not must and (played >= 8 or not ready(item, it)):
                    break
                P.play(item)
                glue = False
                played += 1
                dpos += 1
        while dpos < len(deferred):
            if deferred[dpos] is not None and deferred[dpos] != "GLUE":
                P.play(deferred[dpos])
            dpos += 1

        if dbg:
            for k in range(8):
                P.dma("sp", lambda e, k=k: e.dma_start(out=dbg_mixT[k, :, tok0:tok0 + W], in_=mixT[:, k, :]), reads=[BmixT[k]])

        if stop == '1c':
            continue
        def layer_norm3(pres, Bpres, nrms, Bnrms, g_rep, b_rep, dsts, Bdsts):
            J = range(len(pres))
            for c in range(2):
                for j in J:
                    P.op("dve", lambda e, c=c, j=j: e.bn_stats(out=st6[:, j, c * 6:(c + 1) * 6], in_=pres[j][:, c * 512:(c + 1) * 512]),
                         Bpres[j], [Bln[j]])
            for j in J:
                P.op("dve", lambda e, j=j: e.bn_aggr(out=mv[:, j, :], in_=st6[:, j, :]), [Bln[j]], [Bln[j]])
            for j in J:
                ts("dve", lnr[:, j, 0:1], mv[:, j, 1:2], LN_EPS, ALU.add, [Bln[j]], [Bln[j]])
            for j in J:
                act(lnr[:, j, 0:1], lnr[:, j, 0:1], AF.Ln, [Bln[j]], [Bln[j]])
            for j in J:
                act(lnr[:, j, 0:1], lnr[:, j, 0:1], AF.Exp, [Bln[j]], [Bln[j]], scale=-0.5)
            for j in J:
                stt(lnr[:, j, 1:2], mv[:, j, 0:1], -1.0, lnr[:, j, 0:1], ALU.mult, ALU.mult, [Bln[j]], [Bln[j]])
            for j in J:
                act(nrms[j], pres[j], AF.Identity, Bpres[j] + [Bln[j]], Bnrms[j], scale=lnr[:, j, 0:1], bias=lnr[:, j, 1:2])
            for j in J:
                tt("dve", nrms[j], nrms[j], g_rep[:], ALU.mult, Bnrms[j] + [Bconst], Bnrms[j])
            for j in J:
                tt(PL, dsts[j], nrms[j], b_rep[:], ALU.add, Bnrms[j] + [Bconst], Bdsts[j])

        xr = [TT[:, 2 * j:2 * j + 2, :].rearrange("p a b -> p (a b)") for j in range(SBT)]
        Bxr = [BT[2 * j:2 * j + 2] for j in range(SBT)]
        for j in range(SBT):
            i = t0 + j
            xres = xr[j]
            Bx_ = Bxr[j]
            if i == 0:
                P.dma("sp", lambda e, xres=xres: e.dma_start(out=xres[0:16, :], in_=meta_d[:, :]), writes=Bx_)
                P.dma("sp", lambda e, xres=xres: e.dma_start(out=xres[16:128, :], in_=x_d[0:112, :]), writes=Bx_)
            elif i < NT - 1:
                P.dma("sp", lambda e, i=i, xres=xres: e.dma_start(out=xres[:, :], in_=x_d[128 * i - 16:128 * i + 112, :]), writes=Bx_)
            else:
                P.op(PL, lambda e, xres=xres: e.memset(xres[:, :], 0.0), [], Bx_)
                P.dma("sp", lambda e, xres=xres: e.dma_start(out=xres[0:16, :], in_=x_d[SEQ - 16:SEQ, :]), writes=Bx_)
        for half in range(2):
            wi = load_wblk(w_out_bf[:, half * 512:(half + 1) * 512].rearrange("(k p) c -> p k c", p=128))
            hs = slice(half * 512, (half + 1) * 512)
            for j in range(SBT):
                b = next_bank(0, 6)
                for k in range(8):
                    mm(psf[b][:, :], mixT[:, k, j * 128:(j + 1) * 128], wblk[wi][:, k, :], k == 0, k == 7,
                       [BmixT[k], Bwblk[wi]], [PB[b]])
                stt(xr[j][:, hs], xr[j][:, hs], ALPHA, psf[b][:, :], ALU.mult, ALU.add, Bxr[j] + [PB[b]], Bxr[j])
        layer_norm3(xr, Bxr, xr, Bxr, ln1g, ln1b, [h1[:, j, :] for j in range(SBT)], [[Bh1[j]] for j in range(SBT)])
        if dbg:
            for j in range(SBT):
                i = t0 + j
                P.dma("sp", lambda e, i=i, j=j: e.dma_start(out=dbg_h1[i * 128:(i + 1) * 128, :], in_=h1[:, j, :]), reads=[Bh1[j]])

        if stop == '1d':
            continue
        if Q == 0:
            issue_cast(NE)
        P.barrier()
        for j in range(SBT):
            for half in range(2):
                for k4 in range(4):
                    k = half * 4 + k4
                    mm(psf[half][:, k4 * 128:(k4 + 1) * 128], h1[:, j, k * 128:(k + 1) * 128], ident_f[:], k4 == 0, k4 == 3,
                       [Bh1[j], Bconst], [PB[half]])
                act(h1T_f[:, half * 4:half * 4 + 4, :], psf[half][:, :].rearrange("p (k t) -> p k t", k=4), AF.Copy,
                    [PB[half]], [Bh1Tf])
                act(h1T_b[:, half * 4:half * 4 + 4, j * 128:(j + 1) * 128], h1T_f[:, half * 4:half * 4 + 4, :], AF.Copy,
                    [Bh1Tf], [Bh1Tb[j]])
            ts(PL, h1[:, j, :], h1[:, j, :], ALPHA, ALU.mult, [Bh1[j]], [Bh1[j]])
            for k in range(8):
                mm(psf[2][:, 0:20], h1T_f[:, k, :], w_r[:, k, :], k == 0, False, [Bh1Tf, Bconst], [PB[2]])
            mm(psf[2][:, 0:20], ones_f[0:1, :], b_r[0:1, :], False, True, [Bconst], [PB[2]])
            P.op("dve", lambda e, j=j: e.tensor_copy(out=lg[:, j, :], in_=psf[2][:, 0:20]), [PB[2]], [Blg[j]])
        if stop in ('2a0', '2a1'):
            continue

        def rc(j, i):
            return rt[:, j, i:i + 1]
        J3 = range(SBT)
        RB = lambda j: [Blg[j], Brt[j]]
        for j in J3:
            P.op(PL, lambda e, j=j: e.memset(rt[:, j, 0:16], 0.0), [], [Brt[j]])
        for j in J3:
            P.op("dve", lambda e, j=j: e.reduce_max(out=rc(j, 0), in_=lg[:, j, 0:4], axis=AX.X), RB(j), [Brt[j]])
        for j in J3:
            ts("dve", rc(j, 1), rc(j, 0), -1.0, ALU.mult, RB(j), [Brt[j]])
        for j in J3:
            ts("dve", rt[:, j, 16:20], lg[:, j, 0:4], rc(j, 0), ALU.is_equal, RB(j), [Brt[j]])
        for j in J3:
            act(rt[:, j, 24:28], lg[:, j, 0:4], AF.Exp, RB(j), [Brt[j]], bias=rc(j, 1), scale=1.0, accum_out=rc(j, 2))
        for j in J3:
            ts("dve", rt[:, j, 20:24], rt[:, j, 16:20], BIG, ALU.mult, RB(j), [Brt[j]], s2=-BIG, op1=ALU.add)
        for j in J3:
            pen = rt[:, j, 20:24]
            pen_bc = bass.AP(tensor=pen.tensor, offset=pen.offset, ap=[list(pen.ap[0]), [1, 4], [0, 4]])
            tt("dve", lem[:, j, :].rearrange("p (g e) -> p g e", g=4), lg[:, j, 4:20].rearrange("p (g e) -> p g e", g=4),
               pen_bc, ALU.add, RB(j), [Brt[j]])
        for j in J3:
            recip(rc(j, 3), rc(j, 2), RB(j), [Brt[j]])
        for j in J3:
            P.op("dve", lambda e, j=j: e.max(out=top8[:, j, :], in_=lem[:, j, :]), [Brt[j]], [Brt[j]])
        for j in J3:
            ts("dve", m1[:, j, :], lem[:, j, :], top8[:, j, 0:1], ALU.is_equal, [Brt[j]], [Brt[j]])
        for j in J3:
            ts("dve", m2[:, j, :], lem[:, j, :], top8[:, j, 1:2], ALU.is_equal, [Brt[j]], [Brt[j]])
        for j in J3:
            tt("dve", rc(j, 4), top8[:, j, 1:2], top8[:, j, 0:1], ALU.subtract, [Brt[j]], [Brt[j]])
        for j in J3:
            act(rc(j, 5), rc(j, 4), AF.Exp, [Brt[j]], [Brt[j]])
        for j in J3:
            ts("dve", rc(j, 6), rc(j, 5), 1.0, ALU.add, [Brt[j]], [Brt[j]])
        for j in J3:
            recip(rc(j, 6), rc(j, 6), [Brt[j]], [Brt[j]])
        for j in J3:
            tt("dve", rc(j, 7), rc(j, 5), rc(j, 6), ALU.mult, [Brt[j]], [Brt[j]])
        for j in J3:
            tt("dve", rc(j, 8), rc(j, 6), rc(j, 3), ALU.mult, [Brt[j]], [Brt[j]])
        for j in J3:
            tt("dve", rc(j, 9), rc(j, 7), rc(j, 3), ALU.mult, [Brt[j]], [Brt[j]])
        for j in J3:
            ts("dve", m1[:, j, :], m1[:, j, :], rc(j, 8), ALU.mult, [Brt[j]], [Brt[j]])
        for j in J3:
            stt(gates[:, j, :], m2[:, j, :], rc(j, 9), m1[:, j, :], ALU.mult, ALU.add, [Brt[j]], [Bgates[j]])
        if stop == '2a':
            continue

        slot = [0]

        def load_expert(e_):
            idx = []
            for src in (w1_bf[e_].rearrange("(k p) c -> p k c", p=128), w3_bf[e_].rearrange("(k p) c -> p k c", p=128)):
                i = slot[0] % 7
                slot[0] += 1
                P.dma("sp", lambda e, i=i, src=src: e.dma_start(out=wslot[i][:], in_=src), reads=[Bexp[e_]], writes=[Bws[i]])
                idx.append(i)
            i = slot[0] % 7
            slot[0] += 1
            dst = wslot[i][:, :, :].rearrange("p a b -> p (a b)").rearrange("p (c n) -> p c n", c=4)
            src = w2_bf[e_].rearrange("(c p) n -> p c n", p=128)
            P.dma("sp", lambda e, dst=dst, src=src: e.dma_start(out=dst, in_=src), reads=[Bexp[e_]], writes=[Bws[i]])
            idx.append(i)
            return idx

        def gate_bcast(e_):
            gi = e_ % 2
            for j in range(SBT):
                di = j % 2
                ts("dve", De[di][:], ident_b[:], gates[:, j, e_:e_ + 1], ALU.mult, [Bconst, Bgates[j]], [BDe[di]])
                mm(psf[3][:, j * 128:(j + 1) * 128], ones_b[:], De[di][:], j == 0, j == SBT - 1, [Bconst, BDe[di]], [PB[3]])
            act(Gb[gi][:], psf[3][:, 0:W], AF.Copy, [PB[3]], [BGb[gi]])

        pbank = [0]

        def hidden_chunk(e_, c, i1, i3, part="AB"):
            gi = e_ % 2
            hi_ = e_ % 2
            if part == "B":
                bi = c % 2
                tt(PL, hidg[hi_][:, c, :], u_sb[bi][:], Gb[gi][:], ALU.mult, [Bu[bi], BGb[gi]], [Bhidg[hi_]])
                return
            bi = pbank[0] % 2
            pbank[0] += 1
            b1, b3 = (4, 5) if bi == 0 else (6, 7)
            for k in range(8):
                mm(psf[b1][:, 0:W], wslot[i1][:, k, c * 128:(c + 1) * 128], h1T_b[:, k, :], k == 0, k == 7,
                   [Bws[i1]] + Bh1Tb, [PB[b1]])
            for k in range(8):
                mm(psf[b3][:, 0:W], wslot[i3][:, k, c * 128:(c + 1) * 128], h1T_b[:, k, :], k == 0, k == 7,
                   [Bws[i3]] + Bh1Tb, [PB[b3]])
            act(s_sb[bi][:], psf[b1][:, 0:W], AF.Silu, [PB[b1]], [Bs[bi]])
            tt("dve", u_sb[bi][:], psf[b3][:, 0:W], s_sb[bi][:], ALU.mult, [PB[b3], Bs[bi]], [Bu[bi]])
            if part == "A":
                return
            tt(PL, hidg[hi_][:, c, :], u_sb[bi][:], Gb[gi][:], ALU.mult, [Bu[bi], BGb[gi]], [Bhidg[hi_]])

        def down_proj(e_, i2):
            hi_ = e_ % 2
            w2v = wslot[i2][:, :, :].rearrange("p a b -> p (a b)")
            for j in range(SBT):
                for half in range(2):
                    b = next_bank(0, 3)
                    for c in range(4):
                        mm(psf[b][:, :], hidg[hi_][:, c, j * 128:(j + 1) * 128],
                           w2v[:, c * 1024 + half * 512:c * 1024 + (half + 1) * 512], c == 0, c == 3,
                           [Bhidg[hi_], Bws[i2]], [PB[b]])
                    hs = slice(half * 512, (half + 1) * 512)
                    tt("dve", h1[:, j, hs], psf[b][:, :], h1[:, j, hs], ALU.add, [PB[b], Bh1[j]], [Bh1[j]])

        widx = {0: load_expert(0)}
        pbank[0] = 0
        for e_ in range(NE):
            i1, i3, i2 = widx[e_]
            hidden_chunk(e_, 0, i1, i3, "A" if e_ == 0 else "AB")
            if e_ + 1 < NE:
                widx[e_ + 1] = load_expert(e_ + 1)
            if e_ > 0:
                down_proj(e_ - 1, widx[e_ - 1][2])
            if e_ == 0:
                hidden_chunk(e_, 1, i1, i3, "A")
                gate_bcast(0)
                hidden_chunk(e_, 0, i1, i3, "B")
                hidden_chunk(e_, 1, i1, i3, "B")
            if e_ + 1 < NE:
                gate_bcast(e_ + 1)
            for c in range(2 if e_ == 0 else 1, 4):
                hidden_chunk(e_, c, i1, i3)
        down_proj(NE - 1, widx[NE - 1][2])
        if stop == '2b':
            continue
        P.barrier()
        ln2_def = []
        P.defer = ln2_def
        nr = [TT[:, 2 * j:2 * j + 2, :].rearrange("p a b -> p (a b)") for j in range(SBT)]
        Bnr = [BT[2 * j:2 * j + 2] for j in range(SBT)]
        layer_norm3([h1[:, j, :] for j in range(SBT)], [[Bh1[j]] for j in range(SBT)], nr, Bnr, ln2g, ln2b, nr, Bnr)
        for j in range(SBT):
            i = t0 + j
            ot = nr[j]
            if i == 0:
                P.dma("sp", lambda e, ot=ot: e.dma_start(out=out_d[0:112, :], in_=ot[16:128, :]), reads=Bnr[j])
            elif i < NT - 1:
                P.dma("sp", lambda e, ot=ot, i=i: e.dma_start(out=out_d[128 * i - 16:128 * i + 112, :], in_=ot[:, :]), reads=Bnr[j])
            else:
                P.dma("sp", lambda e, ot=ot: e.dma_start(out=out_d[SEQ - 16:SEQ, :], in_=ot[0:16, :]), reads=Bnr[j])
        P.defer = None
        pending_ln2[0] = ln2_def
        if Q == min(NSB, maxq) - 1:
            flush_ln2(len(ln2_def))

    P.emit()
    return nc


_CACHE = {}


def _prep_inputs(inputs, b):
    f = lambda a: np.ascontiguousarray(np.asarray(a, dtype=np.float32))
    m = {
        "x": f(inputs["x"][b]),
        "meta_tokens": f(inputs["meta_tokens"]),
        "w_in": f(inputs["w_in"][0]),
        "hg_lower_bound": f(inputs["hg_lower_bound"]),
        "hg_norm_g": f(inputs["hg_norm_g"]),
        "sb_norm_g": f(inputs["sb_norm_g"]),
        "w_out": f(inputs["w_out"][0]),
        "ln1_g": f(inputs["ln1_g"]), "ln1_b": f(inputs["ln1_b"]),
        "w_router_group": f(inputs["w_router_group"][0]),
        "b_router_group": f(inputs["b_router_group"]),
        "w_router_expert": f(np.asarray(inputs["w_router_expert"][0]).reshape(D, 16)),
        "b_router_expert": f(np.asarray(inputs["b_router_expert"]).reshape(1, 16)),
        "w_exp_gate": f(inputs["w_exp_gate"][0]),
        "w_exp_up": f(inputs["w_exp_up"][0]),
        "w_exp_down": f(inputs["w_exp_down"][0]),
        "ln2_g": f(inputs["ln2_g"]), "ln2_b": f(inputs["ln2_b"]),
    }
    m.update(host_consts())
    return m


def kernel(**inputs):
    nc = bass.Bass("TRN2", target_bir_lowering=False)
    build(nc)
    n = 8
    in_maps = [_prep_inputs(inputs, b) for b in range(n)]
    res = run_bass_kernel_spmd(nc, in_maps, core_ids=list(range(n)))
    return np.stack([np.asarray(r["out"], dtype=np.float32) for r in res.results], axis=0)
```

```python
import os
import numpy as np
import concourse.bass as bass
import concourse.mybir as mybir
from concourse.bass_utils import run_bass_kernel_spmd

F32 = mybir.dt.float32
BF16 = mybir.dt.bfloat16
AF = mybir.ActivationFunctionType
ALU = mybir.AluOpType
AX = mybir.AxisListType

D = 1024
SEQ = 4096
NMETA = 16
L = SEQ + NMETA
NT = 33
LP = NT * 128
SBT = 3
NSB = NT // SBT
W = SBT * 128
NE = 16
ALPHA = 2 ** 0.25
LN_EPS = 1e-5
RMS_EPS = 1e-6
BIG = 1.0e4
SAME_ENGINE_SKIP = 10 ** 9

ENGS = ("pe", "act", "dve", "pool", "sp")


class Buf:
    __slots__ = ("name", "writers", "readers", "dsem", "dcount")

    def __init__(self, name):
        self.name = name
        self.writers = {}
        self.readers = {}
        self.dsem = None
        self.dcount = 0


class Prog:
    def __init__(self, nc):
        self.nc = nc
        self.ops = {e: [] for e in ENGS}
        self.nsem = 0
        self.dma_bufs = []
        self._dsem_of = {}
        self._keep = []
        self.defer = None

    def play(self, item):
        kind, eng, fn, reads, writes = item
        if kind == "op":
            self.op(eng, fn, list(reads), list(writes))
        else:
            self.dma(eng, fn, list(reads), list(writes))

    def _new_dma_sem(self):
        cm = self.nc.semaphore("dq%d" % self.nsem)
        self.nsem += 1
        h = cm.__enter__()
        self._keep.append(cm)
        return h

    def _collect(self, eng, reads, writes):
        deps = {}

        here = len(self.ops[eng])

        def add(k, v):
            if k == eng and eng == "pe":
                return
            if k == eng and eng in ("dve", "act") and here - v >= SAME_ENGINE_SKIP:
                return
            if deps.get(k, -1) < v:
                deps[k] = v
        for b in reads:
            for k, v in b.writers.items():
                add(k, v)
        for b in writes:
            for k, v in b.readers.items():
                add(k, v)
            for k, v in b.writers.items():
                add(k, v)
        return deps

    def _update(self, key, val, reads, writes):
        for b in reads:
            if b.readers.get(key, -1) < val:
                b.readers[key] = val
        for b in writes:
            if b.readers:
                b.readers = {}
                b.writers = {}
            if b.writers.get(key, -1) < val:
                b.writers[key] = val

    def op(self, eng, fn, reads=(), writes=()):
        if self.defer is not None:
            self.defer.append(("op", eng, fn, tuple(reads), tuple(writes)))
            return
        deps = self._collect(eng, reads, writes)
        pos = len(self.ops[eng])
        self.ops[eng].append({"deps": deps, "fn": fn, "dma": None, "need_inc": False})
        self._update(eng, pos, reads, writes)

    def dma(self, eng, fn, reads=(), writes=(), n=1):
        if self.defer is not None:
            self.defer.append(("dma", eng, fn, tuple(reads), tuple(writes)))
            return
        deps = self._collect(eng, reads, writes)
        owner = writes[0] if writes else reads[0]
        cls = "sw" if eng == "pool" else "hw"
        if owner.dsem is None:
            owner.dsem = {}
        if cls not in owner.dsem:
            owner.dsem[cls] = [self._new_dma_sem(), 0]
            self.dma_bufs.append(owner.dsem[cls])
        ent = owner.dsem[cls]
        ent[1] += 16 * n
        key = ("dma", id(owner), cls)
        self._dsem_of[key] = ent[0]
        self.ops[eng].append({"deps": deps, "fn": fn, "dma": ent[0], "need_inc": False})
        self._update(key, ent[1], reads, writes)

    def barrier(self):
        last = {}
        for e in ENGS:
            idx = [i for i, o in enumerate(self.ops[e]) if o["fn"] is not None and o["dma"] is None]
            if idx:
                last[e] = idx[-1]
        for e in ENGS:
            deps = {k: v for k, v in last.items() if k != e}
            self.ops[e].append({"deps": deps, "fn": None, "dma": None, "need_inc": False})

    def emit(self):
        nc = self.nc
        for e in ENGS:
            for o in self.ops[e]:
                for k, v in o["deps"].items():
                    if isinstance(k, str):
                        self.ops[k][v]["need_inc"] = True
        cnt_at = {}
        for e in ENGS:
            c = 0
            arr = []
            for o in self.ops[e]:
                if o["need_inc"]:
                    assert o["fn"] is not None and o["dma"] is None
                    c += 1
                arr.append(c)
            cnt_at[e] = arr
        sems = {}
        for e in ENGS:
            if e == "sp":
                continue
            cm = nc.semaphore("c_" + e)
            sems[e] = cm.__enter__()
            self._keep.append(cm)
        handles = {"pe": "tensor", "act": "scalar", "dve": "vector", "pool": "gpsimd", "sp": "sync"}
        with nc.Block() as block:
            for e in ENGS:
                ops = self.ops[e]

                def body(eh, e=e, ops=ops):
                    seen = {}
                    for o in ops:
                        for k, v in o["deps"].items():
                            if isinstance(k, str):
                                sem = sems[k]
                                val = cnt_at[k][v]
                            else:
                                sem = self._dsem_of[k]
                                val = v
                            if seen.get(sem.num, -1) >= val:
                                continue
                            seen[sem.num] = val
                            eh.wait_ge(sem, val)
                        if o["fn"] is None:
                            continue
                        ins = o["fn"](eh)
                        if o["dma"] is not None:
                            ins.then_inc(o["dma"], 16)
                        elif o["need_inc"]:
                            ins.then_inc(sems[e], 1)
                    if e == "sp":
                        for ent in self.dma_bufs:
                            eh.wait_ge(ent[0], ent[1])
                getattr(block, handles[e])(body)


def host_consts():
    s = np.arange(128)
    same = (s[:, None] // 64) == (s[None, :] // 64)
    c = {}
    c["c_ident"] = np.eye(128, dtype=np.float32)
    c["c_btri"] = (same & (s[:, None] <= s[None, :])).astype(np.float32)
    c["c_negtri"] = -(s[:, None] >= s[None, :]).astype(np.float32)
    c["c_stri"] = (s[:, None] < s[None, :]).astype(np.float32)
    c["c_bones"] = same.astype(np.float32)
    c["c_ones"] = np.ones((128, 128), np.float32)
    ci = np.zeros((128, 2), np.float32)
    ci[:64, 0] = 1.0
    ci[64:, 1] = 1.0
    c["c_cind"] = ci
    return c


def build(nc, dbg=False, maxq=NSB, stop=None, nexp=NE):
    P = Prog(nc)

    def dram_in(name, shape):
        return nc.dram_tensor(name, list(shape), F32, kind="ExternalInput").ap()

    x_d = dram_in("x", [SEQ, D])
    meta_d = dram_in("meta_tokens", [NMETA, D])
    w_in_d = dram_in("w_in", [D, 3584])
    lbp_d = dram_in("hg_lower_bound", [2, 512])
    hgg_d = dram_in("hg_norm_g", [1, 512])
    sbg_d = dram_in("sb_norm_g", [1, 512])
    w_out_d = dram_in("w_out", [D, D])
    ln1g_d = dram_in("ln1_g", [1, D])
    ln1b_d = dram_in("ln1_b", [1, D])
    wrg_d = dram_in("w_router_group", [D, 4])
    brg_d = dram_in("b_router_group", [1, 4])
    wre_d = dram_in("w_router_expert", [D, 16])
    bre_d = dram_in("b_router_expert", [1, 16])
    w1_d = dram_in("w_exp_gate", [NE, D, 512])
    w3_d = dram_in("w_exp_up", [NE, D, 512])
    w2_d = dram_in("w_exp_down", [NE, 512, D])
    ln2g_d = dram_in("ln2_g", [1, D])
    ln2b_d = dram_in("ln2_b", [1, D])
    cst = {k: dram_in(k, v.shape) for k, v in host_consts().items()}
    out_d = nc.dram_tensor("out", [SEQ, D], F32, kind="ExternalOutput").ap()
    if dbg:
        dbg_mixT = nc.dram_tensor("dbg_mixT", [8, 128, LP], BF16, kind="ExternalOutput").ap()
        dbg_h1 = nc.dram_tensor("dbg_h1", [LP, D], F32, kind="ExternalOutput").ap()

    w_in_bf = nc.dram_tensor("w_in_bf", [D, 3584], BF16).ap()
    w_out_bf = nc.dram_tensor("w_out_bf", [D, D], BF16).ap()
    w1_bf = nc.dram_tensor("w1_bf", [NE, D, 512], BF16).ap()
    w3_bf = nc.dram_tensor("w3_bf", [NE, D, 512], BF16).ap()
    w2_bf = nc.dram_tensor("w2_bf", [NE, 512, D], BF16).ap()

    sb_lo = (nc.sbuf_base + 63) // 64 * 64
    sb_hi = nc.sbuf_top
    cur = [sb_lo]
    cnt = [0]

    def sb(shape, dt):
        n = 1
        for s_ in shape[1:]:
            n *= s_
        nbytes = n * (4 if dt == F32 else 2)
        nbytes = (nbytes + 63) // 64 * 64
        off = cur[0]
        cur[0] += nbytes
        assert cur[0] <= sb_hi, ("SBUF overflow", cur[0], sb_hi)
        cnt[0] += 1
        return nc.alloc_sbuf_tensor_at("t%d" % cnt[0], list(shape), dt, offset=off)

    psf = [nc.alloc_psum_tensor("psf%d" % i, [128, 512], F32) for i in range(8)]
    nc.psum_base = 0
    psb = [nc.alloc_psum_tensor("psb%d" % i, [128, 1024], BF16) for i in range(8)]
    PB = [Buf("bank%d" % i) for i in range(8)]

    kT = sb([128, 4, LP], BF16)
    vv = sb([128, NT, 512], BF16)
    BkT = [Buf("kT%d" % i) for i in range(NSB)]
    Bv = [Buf("v%d" % i) for i in range(NSB)]
    ident_b = sb([128, 128], BF16)
    ident_f = sb([128, 128], F32)
    btri_f = sb([128, 128], F32)
    btri_b = sb([128, 128], BF16)
    negtri_b = sb([128, 128], BF16)
    stri_b = sb([128, 128], BF16)
    bones_b = sb([128, 128], BF16)
    ones_b = sb([128, 128], BF16)
    ones_f = sb([128, 128], F32)
    cind_f = sb([128, 2], F32)
    ln1g = sb([128, D], F32)
    ln1b = sb([128, D], F32)
    ln2g = sb([128, D], F32)
    ln2b = sb([128, D], F32)
    lb_rep = sb([128, 512], F32)
    oml_rep = sb([128, 512], F32)
    hgg_rep = sb([128, 512], F32)
    sbgT = sb([128, 4], F32)
    w_r = sb([128, 8, 20], F32)
    b_r = sb([1, 20], F32)
    S_f = sb([128, 4, 128], F32)
    S_b = sb([128, 4, 128], BF16)
    tmpS = [sb([128, 128], F32) for _ in range(2)]
    h1 = sb([128, SBT, D], F32)
    TT = sb([128, 8, 512], F32)
    st6 = sb([128, SBT, 12], F32)
    mv = sb([128, SBT, 2], F32)
    lnr = sb([128, SBT, 2], F32)
    Bln = [Buf("ln%d" % j) for j in range(SBT)]
    Bconst = Buf("const")
    BS = [Buf("S%d" % h) for h in range(4)]
    BtmpS = [Buf("tmpS0"), Buf("tmpS1")]
    Bh1 = [Buf("h1_%d" % j) for j in range(SBT)]
    BT = [Buf("T%d" % i) for i in range(8)]

    region_lo = cur[0]

    xb = sb([128, D], BF16)
    hT = sb([128, 8, W], BF16)
    wblk = [sb([128, 8, 512], BF16) for _ in range(2)]
    hq_sb = sb([128, SBT, 512], BF16)
    hf_sb = sb([128, SBT, 512], F32)
    hi_sb = sb([128, SBT, 512], BF16)
    hg_sb = sb([128, SBT, 512], BF16)
    qT = sb([128, 4, W], BF16)
    mixT = sb([128, 8, W], BF16)
    qtl = sb([128, 512], BF16)
    ktl = sb([128, 512], BF16)
    qkT = sb([128, 8, 128], BF16)
    At = sb([128, 4, 128], BF16)
    mx = sb([128, 512], BF16)
    el = sb([128, 8], F32)
    ssq = sb([128, 4], F32)
    rstd4 = sb([128, 4], F32)
    Lp = [[sb([128, W], BF16) for _ in range(2)] for _ in range(2)]
    Aa = [[sb([128, W], BF16) for _ in range(2)] for _ in range(2)]
    r_sb = [sb([1, W], BF16) for _ in range(4)]
    Ef = [[sb([128, W], F32) for _ in range(3)] for _ in range(2)]
    Osb_t = sb([128, W], F32)
    rs_t = sb([128, W], F32)
    Xf = [sb([128, W], F32) for _ in range(2)]
    sqb = sb([128, W], BF16)
    Bxb = Buf("xb"); BhT = Buf("hT"); Bwblk = [Buf("wblk0"), Buf("wblk1")]
    Bhq = [Buf("hq%d" % j) for j in range(SBT)]
    Bhf = [Buf("hf%d" % j) for j in range(SBT)]
    Bhi = [Buf("hi%d" % j) for j in range(SBT)]
    Bhg = [Buf("hg%d" % j) for j in range(SBT)]
    BqT = Buf("qT")
    BmixT = [Buf("mixT%d" % k) for k in range(8)]
    Bqtl = Buf("qtl"); Bktl = Buf("ktl"); BqkT = Buf("qkT"); BAt = Buf("At"); Bmx = Buf("mx")
    Bel = Buf("el"); Bssq = Buf("ssq"); Brstd4 = Buf("rstd4")
    BLp = [[Buf("Lp%d%d" % (a_, b_)) for b_ in range(2)] for a_ in range(2)]; BAa = [[Buf("A%d%d" % (a_, b_)) for b_ in range(2)] for a_ in range(2)]; Br = [Buf("r%d" % i) for i in range(4)]; BEf = [[Buf("Ef%d%d" % (a_, b_)) for b_ in range(3)] for a_ in range(2)]; BXf = [Buf("Xf0"), Buf("Xf1")]; BOsb = Buf("Osb"); Brs = Buf("rs")
    Bsqb = Buf("sqb")
    p1_hi = cur[0]

    cur[0] = region_lo
    h1T_f = sb([128, 8, 128], F32)
    h1T_b = sb([128, 8, W], BF16)
    wslot = [sb([128, 8, 512], BF16) for _ in range(7)]
    lg = sb([128, SBT, 20], F32)
    rt = sb([128, SBT, 64], F32)
    lem = sb([128, SBT, 16], F32)
    top8 = sb([128, SBT, 8], F32)
    m1 = sb([128, SBT, 16], F32)
    m2 = sb([128, SBT, 16], F32)
    gates = sb([128, SBT, 16], F32)
    De = [sb([128, 128], BF16) for _ in range(2)]
    Gb = [sb([128, W], BF16) for _ in range(2)]
    s_sb = [sb([128, W], BF16) for _ in range(2)]
    u_sb = [sb([128, W], BF16) for _ in range(2)]
    hidg = [sb([128, 4, W], BF16) for _ in range(2)]
    Bh1Tf = Buf("h1Tf"); Bh1Tb = [Buf("h1Tb%d" % j) for j in range(SBT)]
    Bws = [Buf("ws%d" % i) for i in range(7)]
    Blg = [Buf("lg%d" % j) for j in range(SBT)]; Brt = [Buf("rt%d" % j) for j in range(SBT)]; Bgates = [Buf("gates%d" % j) for j in range(SBT)]
    BDe = [Buf("De0"), Buf("De1")]; BGb = [Buf("Gb0"), Buf("Gb1")]
    Bs = [Buf("s0"), Buf("s1")]; Bu = [Buf("u0"), Buf("u1")]; Bhidg = [Buf("hidg0"), Buf("hidg1")]
    p2_hi = cur[0]
    cur[0] = max(p1_hi, p2_hi)

    def mm(out, lhsT, rhs, start, stop, reads, writes):
        P.op("pe", lambda e: e.matmul(out, lhsT=lhsT, rhs=rhs, start=start, stop=stop, skip_group_check=True),
             reads, writes)

    def tr(out, in_, ident, reads, writes):
        P.op("pe", lambda e: e.transpose(out=out, in_=in_, identity=ident), reads, writes)

    def act(out, in_, func, reads, writes, **kw):
        P.op("act", lambda e: e.activation(out=out, in_=in_, func=func, **kw), reads, writes)

    def tt(eng, out, in0, in1, op, reads, writes):
        P.op(eng, lambda e: e.tensor_tensor(out=out, in0=in0, in1=in1, op=op), reads, writes)

    def ts(eng, out, in0, s1, op0, reads, writes, s2=None, op1=None):
        if op1 is None:
            P.op(eng, lambda e: e.tensor_scalar(out=out, in0=in0, scalar1=s1, scalar2=None, op0=op0), reads, writes)
        else:
            P.op(eng, lambda e: e.tensor_scalar(out=out, in0=in0, scalar1=s1, scalar2=s2, op0=op0, op1=op1),
                 reads, writes)

    def stt(out, in0, scalar, in1, op0, op1, reads, writes):
        P.op("dve", lambda e: e.scalar_tensor_tensor(out=out, in0=in0, scalar=scalar, in1=in1, op0=op0, op1=op1),
             reads, writes)

    def recip(out, in_, reads, writes):
        P.op("dve", lambda e: e.reciprocal(out=out, in_=in_), reads, writes)

    flip = [0]

    def evac(out, in_, reads, writes, scale=None):
        flip[0] ^= 1
        if flip[0]:
            if scale is None:
                act(out, in_, AF.Copy, reads, writes)
            else:
                act(out, in_, AF.Copy, reads, writes, scale=scale)
        else:
            if scale is None:
                P.op("dve", lambda e: e.tensor_copy(out=out, in_=in_), reads, writes)
            else:
                ts("dve", out, in_, scale, ALU.mult, reads, writes)

    def bcast_rows(src_ap_1xn, n):
        return bass.AP(tensor=src_ap_1xn.tensor, offset=src_ap_1xn.offset, ap=[[0, 128], [1, n]])

    Bwin = Buf("w_in_bf"); Bwout = Buf("w_out_bf"); Bexp = [Buf("exp%d" % e) for e in range(NE)]
    for r in range(8):
        P.dma("pool", lambda e, r=r: e.dma_start(out=w_in_bf[r * 128:(r + 1) * 128, :], in_=w_in_d[r * 128:(r + 1) * 128, :]),
              writes=[Bwin])
    for r in range(4):
        P.dma("pool", lambda e, r=r: e.dma_start(out=w_out_bf[r * 256:(r + 1) * 256, :], in_=w_out_d[r * 256:(r + 1) * 256, :]),
              writes=[Bwout])
    for nm, ap in (("c_ident", ident_b), ("c_btri", btri_b), ("c_negtri", negtri_b), ("c_stri", stri_b),
                   ("c_bones", bones_b), ("c_ones", ones_b)):
        P.dma("pool", lambda e, nm=nm, ap=ap: e.dma_start(out=ap[:], in_=cst[nm]), writes=[Bconst])
    for nm, ap in (("c_ident", ident_f), ("c_btri", btri_f), ("c_ones", ones_f), ("c_cind", cind_f)):
        P.dma("sp", lambda e, nm=nm, ap=ap: e.dma_start(out=ap[:], in_=cst[nm]), writes=[Bconst])
    for src, dst, n in ((ln1g_d, ln1g, D), (ln1b_d, ln1b, D), (ln2g_d, ln2g, D), (ln2b_d, ln2b, D),
                        (hgg_d, hgg_rep, 512), (lbp_d[0:1, :], lb_rep, 512), (lbp_d[1:2, :], oml_rep, 512)):
        P.dma("sp", lambda e, src=src, dst=dst, n=n: e.dma_start(out=dst[:], in_=bcast_rows(src, n)), writes=[Bconst])
    P.dma("sp", lambda e: e.dma_start(out=sbgT[:], in_=sbg_d.rearrange("o (p q) -> q (o p)", q=128), allow_slow_non_contiguous=True), writes=[Bconst])
    P.dma("sp", lambda e: e.dma_start(out=w_r[:, :, 0:4], in_=wrg_d.rearrange("(k p) g -> p k g", p=128), allow_slow_non_contiguous=True), writes=[Bconst])
    P.dma("sp", lambda e: e.dma_start(out=w_r[:, :, 4:20], in_=wre_d.rearrange("(k p) g -> p k g", p=128), allow_slow_non_contiguous=True), writes=[Bconst])
    P.dma("sp", lambda e: e.dma_start(out=b_r[:, 0:4], in_=brg_d), writes=[Bconst])
    P.dma("sp", lambda e: e.dma_start(out=b_r[:, 4:20], in_=bre_d), writes=[Bconst])
    cast_next = [0]

    def issue_cast(n=1):
        for _ in range(n):
            e_ = cast_next[0]
            if e_ >= nexp:
                return
            cast_next[0] += 1
            thr = [Bexp[e_ - 2]] if e_ >= 2 else [Bwout, Bconst]
            for (src, dst) in ((w1_d, w1_bf), (w3_d, w3_bf)):
                for hh in range(2):
                    P.dma("pool", lambda e, src=src, dst=dst, e_=e_, hh=hh: e.dma_start(
                        out=dst[e_, hh * 512:(hh + 1) * 512, :], in_=src[e_, hh * 512:(hh + 1) * 512, :]),
                        reads=thr, writes=[Bexp[e_]])
            for hh in range(2):
                P.dma("pool", lambda e, e_=e_, hh=hh: e.dma_start(
                    out=w2_bf[e_, hh * 256:(hh + 1) * 256, :], in_=w2_d[e_, hh * 256:(hh + 1) * 256, :]),
                    reads=thr, writes=[Bexp[e_]])

    issue_cast(NE)
    tt("dve", oml_rep[:], oml_rep[:], lb_rep[:], ALU.subtract, [Bconst], [Bconst])
    act(oml_rep[:], oml_rep[:], AF.Exp, [Bconst], [Bconst])
    ts("dve", lb_rep[:], oml_rep[:], 1.0, ALU.add, [Bconst], [Bconst])
    recip(lb_rep[:], lb_rep[:], [Bconst], [Bconst])
    tt("dve", oml_rep[:], oml_rep[:], lb_rep[:], ALU.mult, [Bconst], [Bconst])
    P.op("dve", lambda e: e.memset(S_f[:], 0.0), [], BS)
    P.op("dve", lambda e: e.memset(S_b[:], 0.0), [], BS)

    def Tf(i, n=512):
        return TT[:, i, 0:n]

    def load_x_rows(eng, dst, i, Bdst):
        if i == 0:
            P.dma(eng, lambda e: e.dma_start(out=dst[0:16, :], in_=meta_d[:, :]), writes=[Bdst])
            P.dma(eng, lambda e: e.dma_start(out=dst[16:128, :], in_=x_d[0:112, :]), writes=[Bdst])
        elif i < NT - 1:
            P.dma(eng, lambda e: e.dma_start(out=dst[:, :], in_=x_d[128 * i - 16:128 * i + 112, :]), writes=[Bdst])
        else:
            P.op("dve", lambda e: e.memset(dst[:, :], 0.0), [], [Bdst])
            P.dma(eng, lambda e: e.dma_start(out=dst[0:16, :], in_=x_d[SEQ - 16:SEQ, :]), writes=[Bdst])

    wctr = [0]

    def load_wblk(src3):
        i = wctr[0] % 2
        wctr[0] += 1
        P.dma("sp", lambda e: e.dma_start(out=wblk[i][:], in_=src3), reads=[Bwin, Bwout], writes=[Bwblk[i]])
        return i

    bankrr = [0]

    def next_bank(lo=0, hi=6):
        b = lo + bankrr[0] % (hi - lo)
        bankrr[0] += 1
        return b

    pending_ln2 = [[]]

    def flush_ln2(n):
        lst = pending_ln2[0]
        for _ in range(min(n, len(lst))):
            P.play(lst.pop(0))

    for Q in range(min(NSB, maxq)):
        t0 = Q * SBT
        PL = "dve" if Q == 0 else "pool"
        tok0 = t0 * 128
        for j in range(SBT):
            xst = TT[:, 6:8, :].rearrange("p a b -> p (a b)")
            load_x_rows("sp", xst, t0 + j, BT[6])
            act(xb[:, :], xst, AF.Copy, [BT[6]], [Bxb])
            for k in range(8):
                tr(psb[7][:, k * 128:(k + 1) * 128], xb[:, k * 128:(k + 1) * 128], ident_b[:], [Bxb, Bconst], [PB[7]])
            evac(hT[:, :, j * 128:(j + 1) * 128], psb[7][:, :].rearrange("p (k t) -> p k t", k=8), [PB[7]], [BhT])
        for cb in (1, 0, 2, 3, 6, 4, 5):
            wi = load_wblk(w_in_bf[:, cb * 512:(cb + 1) * 512].rearrange("(k p) c -> p k c", p=128))
            if cb in (4, 5):
                for p in range(4):
                    b = next_bank()
                    for k in range(8):
                        mm(psf[b][:, 0:W], wblk[wi][:, k, p * 128:(p + 1) * 128], hT[:, k, :], k == 0, k == 7,
                           [Bwblk[wi], BhT], [PB[b]])
                    if cb == 4:
                        evac(qT[:, p, :], psf[b][:, 0:W], [PB[b]], [BqT], scale=0.125)
                    else:
                        evac(kT[:, p, tok0:tok0 + W], psf[b][:, 0:W], [PB[b]], [BkT[Q]])
                    flush_ln2(2)
            else:
                for j in range(SBT):
                    b = next_bank()
                    for k in range(8):
                        mm(psf[b][:, :], hT[:, k, j * 128:(j + 1) * 128], wblk[wi][:, k, :], k == 0, k == 7,
                           [Bwblk[wi], BhT], [PB[b]])
                    if cb == 0:
                        evac(hq_sb[:, j, :], psf[b][:, :], [PB[b]], [Bhq[j]])
                    elif cb == 1:
                        evac(hf_sb[:, j, :], psf[b][:, :], [PB[b]], [Bhf[j]])
                    elif cb == 2:
                        evac(hi_sb[:, j, :], psf[b][:, :], [PB[b]], [Bhi[j]])
                    elif cb == 3:
                        evac(hg_sb[:, j, :], psf[b][:, :], [PB[b]], [Bhg[j]])
                    else:
                        evac(vv[:, t0 + j, :], psf[b][:, :], [PB[b]], [Bv[Q]])
                    flush_ln2(2)

        flush_ln2(10 ** 6)
        if stop == '1a':
            continue
        deferred = []
        P.defer = deferred
        for j in range(SBT):
            t1, t2, t3, t4, t5, t6, t7 = (Tf(i) for i in range(7))
            B1, B2, B3, B4, B5, B6, B7 = BT[0:7]
            if Q == 0:
                issue_cast(1)
            act(t1, hf_sb[:, j, :], AF.Sigmoid, [Bhf[j]], [B1])
            deferred.append("GLUE")
            act(t6, hq_sb[:, j, :], AF.Sigmoid, [Bhq[j]], [B6])
            deferred.append("GLUE")
            act(t7, hg_sb[:, j, :], AF.Sigmoid, [Bhg[j]], [B7])
            tt("dve", t1, t1, oml_rep[:], ALU.mult, [B1, Bconst], [B1])
            tt("dve", t6, t6, hq_sb[:, j, :], ALU.mult, [B6, Bhq[j]], [B6])
            tt("dve", t7, t7, hgg_rep[:], ALU.mult, [B7, Bconst], [B7])
            tt("dve", t1, t1, lb_rep[:], ALU.add, [B1, Bconst], [B1])
            act(t2, t1, AF.Ln, [B1], [B2])
            ts("dve", t3, t1, -1.0, ALU.mult, [B1], [B3], s2=1.0, op1=ALU.add)
            o_sb = Tf(7)
            for h in range(4):
                mm(psf[7][:, 2 * h:2 * h + 2], t2[:, h * 128:(h + 1) * 128], cind_f[:], h == 0, h == 3,
                   [Bconst, B2], [PB[7]])
            act(el[:], psf[7][:, 0:8], AF.Exp, [PB[7]], [Bel])
            deferred.append(None)
            mm(psf[7][:, :], btri_f[:], t2, True, True, [Bconst, B2], [PB[7]])
            act(t4, psf[7][:, :], AF.Exp, [PB[7]], [B4])
            act(t5, psf[7][:, :], AF.Exp, [PB[7]], [B5], scale=-1.0)
            deferred.append(None)
            tt("dve", qtl[:], t6, t4, ALU.mult, [B6, B4], [Bqtl])
            tt("dve", ktl[:], t3, t5, ALU.mult, [B3, B5], [Bktl])
            for h in range(4):
                tr(psb[7][:, h * 128:(h + 1) * 128], qtl[:, h * 128:(h + 1) * 128], ident_b[:], [Bqtl, Bconst], [PB[7]])
            for h in range(4):
                tr(psb[7][:, (4 + h) * 128:(5 + h) * 128], ktl[:, h * 128:(h + 1) * 128], ident_b[:], [Bktl, Bconst], [PB[7]])
            evac(qkT[:, :, :], psb[7][:, :].rearrange("p (k t) -> p k t", k=8), [PB[7]], [BqkT])
            deferred.append(None)
            for h in range(4):
                mm(psf[7][:, h * 128:(h + 1) * 128], qkT[:, 4 + h, :], qkT[:, h, :], h == 0, h == 3, [BqkT], [PB[7]])
            btri_bc = bass.AP(tensor=btri_b, offset=0, ap=[[128, 128], [0, 4], [1, 128]])
            tt("dve", At[:, :, :], psf[7][:, :].rearrange("p (h t) -> p h t", h=4), btri_bc, ALU.mult,
               [PB[7], Bconst], [BAt])
            deferred.append(None)
            for h in range(4):
                hs = slice(h * 128, (h + 1) * 128)
                mm(psf[7][:, hs], At[:, h, :], hi_sb[:, j, hs], h == 0, h == 3, [BAt, Bhi[j]], [PB[7]])
            evac(o_sb, psf[7][:, :], [PB[7]], [BT[7]])
            deferred.append(None)
            for c in range(2):
                rows = slice(64 * c, 64 * c + 64)
                for h in range(4):
                    hs = slice(h * 128, (h + 1) * 128)
                    mm(psf[7][rows, hs], qkT[:, h, rows], S_b[:, h, :], h == 0, h == 3, [BqkT, BS[h]], [PB[7]])
                tt("dve", o_sb[rows, :], psf[7][rows, :], o_sb[rows, :], ALU.add, [PB[7], BT[7]], [BT[7]])
                deferred.append(None)
                for h in range(4):
                    hs = slice(h * 128, (h + 1) * 128)
                    mm(psf[7][:, hs], ktl[rows, hs], hi_sb[rows, j, hs], h == 0, h == 3, [Bktl, Bhi[j]], [PB[7]])
                S_f2 = S_f[:, :, :].rearrange("p h d -> p (h d)")
                tt("dve", t2, psf[7][:, :], S_f2, ALU.add, [PB[7]] + BS, [B2])
                deferred.append(None)
                el_bc = bass.AP(tensor=el, offset=c, ap=[[8, 128], [2, 4], [0, 128]])
                t2v = t2.rearrange("p (h d) -> p h d", h=4)
                tt("dve", S_b[:, :, :], t2v, el_bc, ALU.mult, [B2, Bel], BS)
                tt("dve", S_f[:, :, :], t2v, el_bc, ALU.mult, [B2, Bel], BS)
            P.op(PL, lambda e: e.memset(ssq[:], 0.0), [], [Bssq])
            for h in range(4):
                hs = slice(h * 128, (h + 1) * 128)
                act(t1[:, hs], o_sb[:, hs], AF.Square, [BT[7]], [B1, Bssq], accum_out=ssq[:, h:h + 1])
            ts("dve", rstd4[:], ssq[:], 1.0 / 128.0, ALU.mult, [Bssq], [Brstd4], s2=RMS_EPS, op1=ALU.add)
            act(rstd4[:], rstd4[:], AF.Ln, [Brstd4], [Brstd4])
            act(rstd4[:], rstd4[:], AF.Exp, [Brstd4], [Brstd4], scale=-0.5)
            for h in range(4):
                hs = slice(h * 128, (h + 1) * 128)
                stt(mx[:, hs], o_sb[:, hs], rstd4[:, h:h + 1], t7[:, hs], ALU.mult, ALU.mult,
                    [BT[7], Brstd4, B7], [Bmx])
            for h in range(4):
                tr(psb[7][:, h * 128:(h + 1) * 128], mx[:, h * 128:(h + 1) * 128], ident_b[:], [Bmx, Bconst], [PB[7]])
            evac(mixT[:, 0:4, j * 128:(j + 1) * 128], psb[7][:, 0:512].rearrange("p (k t) -> p k t", k=4),
                 [PB[7]], BmixT[0:4])
            deferred.append(None)

        if stop == '1b':
            for it_ in deferred:
                if it_ is not None and it_ != "GLUE":
                    P.play(it_)
            continue
        P.defer = None
        jmax = t0 + SBT - 1
        Wq = min(W, L - tok0)
        groups = [(p, j) for p in range(4) for j in range(jmax, -1, -1)]
        ng = len(groups)
        PRS = (slice(0, 64), slice(64, 128))

        def ginfo(g):
            p, j = groups[g]
            c0 = 128 * max(0, j - t0)
            return dict(p=p, j=j, c0=c0, cs=slice(c0, Wq), dg=slice(c0, min(c0 + 128, Wq)), dn=min(c0 + 128, Wq) - c0,
                        first=(j == jmax), Qi=j // SBT, ob=4,
                        e3=g % 3, s2=g % 2, ri=[(p % 2) * 2, (p % 2) * 2 + 1],
                        cb=[2, 3] if g % 2 == 0 else [5, 6])

        def sA(g):
            t = ginfo(g)
            cs = t["cs"]
            if t["first"]:
                for hf_ in range(2):
                    P.op(PL, lambda e, ri=t["ri"][hf_]: e.memset(r_sb[ri][:], 0.0), [], [Br[t["ri"][hf_]]])
            for hf_ in range(2):
                mm(psf[hf_][:, cs], kT[PRS[hf_], t["p"], t["j"] * 128:(t["j"] + 1) * 128], qT[PRS[hf_], t["p"], cs], True, True,
                   [BkT[t["Qi"]], BqT], [PB[0], PB[1]] if hf_ == 0 else [PB[1]])

        def sB(g):
            t = ginfo(g)
            cs, c0, e3, s2 = t["cs"], t["c0"], t["e3"], t["s2"]
            for hf_ in range(2):
                act(Ef[hf_][e3][:, cs], psf[hf_][:, cs], AF.Exp, [PB[hf_]], [BEf[hf_][e3]])
            for hf_ in range(2):
                act(Lp[hf_][s2][:, cs], Ef[hf_][e3][:, cs], AF.Ln, [BEf[hf_][e3]], [BLp[hf_][s2]], bias=1.0)
            if t["j"] >= t0:
                for hf_ in range(2):
                    tt("dve", Lp[hf_][s2][:, t["dg"]], Lp[hf_][s2][:, t["dg"]], stri_b[:, 0:t["dn"]], ALU.mult,
                       [BLp[hf_][s2], Bconst], [BLp[hf_][s2]])

        def sC(g):
            t = ginfo(g)
            cs, s2, cb = t["cs"], t["s2"], t["cb"]
            for hf_ in range(2):
                mm(psf[cb[hf_]][:, cs], negtri_b[:], Lp[hf_][s2][:, cs], True, t["first"],
                   [Bconst, BLp[0][s2], BLp[1][s2]] if hf_ == 0 else [Bconst, BLp[1][s2]],
                   [PB[cb[0]], PB[cb[1]]] if hf_ == 0 else [PB[cb[1]]])
            if not t["first"]:
                for hf_ in range(2):
                    ri = t["ri"][hf_]
                    mm(psf[cb[hf_]][:, cs], ones_b[0:1, :], r_sb[ri][0:1, cs], False, True,
                       [Bconst, Br[t["ri"][0]], Br[t["ri"][1]]] if hf_ == 0 else [Bconst, Br[ri]], [PB[cb[hf_]]])

        def sD(g):
            t = ginfo(g)
            cs, c0, e3, s2, cb = t["cs"], t["c0"], t["e3"], t["s2"], t["cb"]
            for hf_ in range(2):
                ri = t["ri"][hf_]
                if t["j"] > 0:
                    P.op("dve", lambda e, ri=ri, hf_=hf_: e.tensor_copy(out=r_sb[ri][0:1, cs], in_=psf[cb[hf_]][0:1, cs]),
                         [PB[cb[hf_]]], [Br[ri]])
                act(Xf[hf_][:, cs], psf[cb[hf_]][:, cs], AF.Exp, [PB[cb[hf_]], Br[ri]], [BXf[hf_]])
            for hf_ in range(2):
                eng_ = "dve" if hf_ == 0 else PL
                tt(eng_, Aa[hf_][s2][:, cs], Ef[hf_][e3][:, cs], Xf[hf_][:, cs], ALU.mult, [BEf[hf_][e3], BXf[hf_]], [BAa[hf_][s2]])
                if t["j"] >= t0:
                    tt(eng_, Aa[hf_][s2][:, t["dg"]], Aa[hf_][s2][:, t["dg"]], stri_b[:, 0:t["dn"]], ALU.mult,
                       [BAa[hf_][s2], Bconst], [BAa[hf_][s2]])

        def sF(g):
            t = ginfo(g)
            cs, ob, p, s2 = t["cs"], t["ob"], t["p"], t["s2"]
            for hf_ in range(2):
                h = 2 * p + hf_
                mm(psf[ob][PRS[hf_], cs], vv[:, t["j"], h * 64:(h + 1) * 64], Aa[hf_][s2][:, cs], t["first"], t["j"] == 0,
                   [Bv[t["Qi"]], BAa[0][s2], BAa[1][s2]] if hf_ == 0 else [Bv[t["Qi"]], BAa[1][s2]], [PB[ob]])
            if t["j"] == 0:
                Osb = Osb_t[:, :]
                rs = rs_t[:, :]
                Osb = Osb_t[:, 0:Wq]
                rs = rs_t[:, 0:Wq]
                act(Osb, psf[ob][:, 0:Wq], AF.Copy, [PB[ob]], [BOsb])
                act(sqb[:, 0:Wq], psf[ob][:, 0:Wq], AF.Square, [PB[ob]], [Bsqb])
                mm(psf[7][:, 0:Wq], bones_b[:], sqb[:, 0:Wq], True, True, [Bconst, Bsqb], [PB[7]])
                ts("dve", rs, psf[7][:, 0:Wq], 1.0 / 64.0, ALU.mult, [PB[7]], [Brs], s2=RMS_EPS, op1=ALU.add)
                act(rs, rs, AF.Ln, [Brs], [Brs])
                act(rs, rs, AF.Exp, [Brs], [Brs], scale=-0.5)
                stt(mixT[:, 4 + p, 0:Wq], Osb, sbgT[:, p:p + 1], rs, ALU.mult, ALU.mult, [BOsb, Brs, Bconst], [BmixT[4 + p]])

        stages = ((sF, 4), (sD, 3), (sC, 2), (sB, 1), (sA, 0))
        nit = ng + 4
        ndef = sum(1 for x in deferred if x is not None and x != "GLUE")
        per_it = -(-ndef // nit)
        in_b7 = [False] * len(deferred)
        open_ = False
        for i_, item in enumerate(deferred):
            if item is None:
                open_ = False
                continue
            if item == "GLUE":
                continue
            in_b7[i_] = open_
            if PB[7] in item[4]:
                open_ = True
        dpos = 0
        snaps = []

        def ready(item, it):
            if it < 1:
                return True
            deps = P._collect(item[1], list(item[3]), list(item[4]))
            lim = snaps[it - 1]
            for k, v in deps.items():
                if isinstance(k, str) and v >= lim[k]:
                    return False
            return True

        for it in range(nit):
            snaps.append({e: len(P.ops[e]) for e in ENGS})
            for fn_, lag in stages:
                g = it - lag
                if 0 <= g < ng:
                    fn_(g)
            played = 0
            glue = False
            forced = False
            while dpos < len(deferred):
                item = deferred[dpos]
                if item is None:
                    dpos += 1
                    forced = False
                    continue
                if item == "GLUE":
                    dpos += 1
                    glue = True
                    continue
                must = glue or in_b7[dpos]
                if not must and (played >= 8 or not ready(item, it)):
                    break
                P.play(item)
                glue = False
                played += 1
                dpos += 1
        while dpos < len(deferred):
            if deferred[dpos] is not None and deferred[dpos] != "GLUE":
                P.play(deferred[dpos])
            dpos += 1

        if dbg:
            for k in range(8):
                P.dma("sp", lambda e, k=k: e.dma_start(out=dbg_mixT[k, :, tok0:tok0 + W], in_=mixT[:, k, :]), reads=[BmixT[k]])

        if stop == '1c':
            continue
        def layer_norm3(pres, Bpres, nrms, Bnrms, g_rep, b_rep, dsts, Bdsts):
            J = range(len(pres))
            for c in range(2):
                for j in J:
                    P.op("dve", lambda e, c=c, j=j: e.bn_stats(out=st6[:, j, c * 6:(c + 1) * 6], in_=pres[j][:, c * 512:(c + 1) * 512]),
                         Bpres[j], [Bln[j]])
            for j in J:
                P.op("dve", lambda e, j=j: e.bn_aggr(out=mv[:, j, :], in_=st6[:, j, :]), [Bln[j]], [Bln[j]])
            for j in J:
                ts("dve", lnr[:, j, 0:1], mv[:, j, 1:2], LN_EPS, ALU.add, [Bln[j]], [Bln[j]])
            for j in J:
                act(lnr[:, j, 0:1], lnr[:, j, 0:1], AF.Ln, [Bln[j]], [Bln[j]])
            for j in J:
                act(lnr[:, j, 0:1], lnr[:, j, 0:1], AF.Exp, [Bln[j]], [Bln[j]], scale=-0.5)
            for j in J:
                stt(lnr[:, j, 1:2], mv[:, j, 0:1], -1.0, lnr[:, j, 0:1], ALU.mult, ALU.mult, [Bln[j]], [Bln[j]])
            for j in J:
                act(nrms[j], pres[j], AF.Identity, Bpres[j] + [Bln[j]], Bnrms[j], scale=lnr[:, j, 0:1], bias=lnr[:, j, 1:2])
            for j in J:
                tt("dve", nrms[j], nrms[j], g_rep[:], ALU.mult, Bnrms[j] + [Bconst], Bnrms[j])
            for j in J:
                tt(PL, dsts[j], nrms[j], b_rep[:], ALU.add, Bnrms[j] + [Bconst], Bdsts[j])

        xr = [TT[:, 2 * j:2 * j + 2, :].rearrange("p a b -> p (a b)") for j in range(SBT)]
        Bxr = [BT[2 * j:2 * j + 2] for j in range(SBT)]
        for j in range(SBT):
            i = t0 + j
            xres = xr[j]
            Bx_ = Bxr[j]
            if i == 0:
                P.dma("sp", lambda e, xres=xres: e.dma_start(out=xres[0:16, :], in_=meta_d[:, :]), writes=Bx_)
                P.dma("sp", lambda e, xres=xres: e.dma_start(out=xres[16:128, :], in_=x_d[0:112, :]), writes=Bx_)
            elif i < NT - 1:
                P.dma("sp", lambda e, i=i, xres=xres: e.dma_start(out=xres[:, :], in_=x_d[128 * i - 16:128 * i + 112, :]), writes=Bx_)
            else:
                P.op(PL, lambda e, xres=xres: e.memset(xres[:, :], 0.0), [], Bx_)
                P.dma("sp", lambda e, xres=xres: e.dma_start(out=xres[0:16, :], in_=x_d[SEQ - 16:SEQ, :]), writes=Bx_)
        for half in range(2):
            wi = load_wblk(w_out_bf[:, half * 512:(half + 1) * 512].rearrange("(k p) c -> p k c", p=128))
            hs = slice(half * 512, (half + 1) * 512)
            for j in range(SBT):
                b = next_bank(0, 6)
                for k in range(8):
                    mm(psf[b][:, :], mixT[:, k, j * 128:(j + 1) * 128], wblk[wi][:, k, :], k == 0, k == 7,
                       [BmixT[k], Bwblk[wi]], [PB[b]])
                stt(xr[j][:, hs], xr[j][:, hs], ALPHA, psf[b][:, :], ALU.mult, ALU.add, Bxr[j] + [PB[b]], Bxr[j])
        layer_norm3(xr, Bxr, xr, Bxr, ln1g, ln1b, [h1[:, j, :] for j in range(SBT)], [[Bh1[j]] for j in range(SBT)])
        if dbg:
            for j in range(SBT):
                i = t0 + j
                P.dma("sp", lambda e, i=i, j=j: e.dma_start(out=dbg_h1[i * 128:(i + 1) * 128, :], in_=h1[:, j, :]), reads=[Bh1[j]])

        if stop == '1d':
            continue
        if Q == 0:
            issue_cast(NE)
        P.barrier()
        for j in range(SBT):
            for half in range(2):
                for k4 in range(4):
                    k = half * 4 + k4
                    mm(psf[half][:, k4 * 128:(k4 + 1) * 128], h1[:, j, k * 128:(k + 1) * 128], ident_f[:], k4 == 0, k4 == 3,
                       [Bh1[j], Bconst], [PB[half]])
                act(h1T_f[:, half * 4:half * 4 + 4, :], psf[half][:, :].rearrange("p (k t) -> p k t", k=4), AF.Copy,
                    [PB[half]], [Bh1Tf])
                act(h1T_b[:, half * 4:half * 4 + 4, j * 128:(j + 1) * 128], h1T_f[:, half * 4:half * 4 + 4, :], AF.Copy,
                    [Bh1Tf], [Bh1Tb[j]])
            ts(PL, h1[:, j, :], h1[:, j, :], ALPHA, ALU.mult, [Bh1[j]], [Bh1[j]])
            for k in range(8):
                mm(psf[2][:, 0:20], h1T_f[:, k, :], w_r[:, k, :], k == 0, False, [Bh1Tf, Bconst], [PB[2]])
            mm(psf[2][:, 0:20], ones_f[0:1, :], b_r[0:1, :], False, True, [Bconst], [PB[2]])
            P.op("dve", lambda e, j=j: e.tensor_copy(out=lg[:, j, :], in_=psf[2][:, 0:20]), [PB[2]], [Blg[j]])
        if stop in ('2a0', '2a1'):
            continue

        def rc(j, i):
            return rt[:, j, i:i + 1]
        J3 = range(SBT)
        RB = lambda j: [Blg[j], Brt[j]]
        for j in J3:
            P.op(PL, lambda e, j=j: e.memset(rt[:, j, 0:16], 0.0), [], [Brt[j]])
        for j in J3:
            P.op("dve", lambda e, j=j: e.reduce_max(out=rc(j, 0), in_=lg[:, j, 0:4], axis=AX.X), RB(j), [Brt[j]])
        for j in J3:
            ts("dve", rc(j, 1), rc(j, 0), -1.0, ALU.mult, RB(j), [Brt[j]])
        for j in J3:
            ts("dve", rt[:, j, 16:20], lg[:, j, 0:4], rc(j, 0), ALU.is_equal, RB(j), [Brt[j]])
        for j in J3:
            act(rt[:, j, 24:28], lg[:, j, 0:4], AF.Exp, RB(j), [Brt[j]], bias=rc(j, 1), scale=1.0, accum_out=rc(j, 2))
        for j in J3:
            ts("dve", rt[:, j, 20:24], rt[:, j, 16:20], BIG, ALU.mult, RB(j), [Brt[j]], s2=-BIG, op1=ALU.add)
        for j in J3:
            pen = rt[:, j, 20:24]
            pen_bc = bass.AP(tensor=pen.tensor, offset=pen.offset, ap=[list(pen.ap[0]), [1, 4], [0, 4]])
            tt("dve", lem[:, j, :].rearrange("p (g e) -> p g e", g=4), lg[:, j, 4:20].rearrange("p (g e) -> p g e", g=4),
               pen_bc, ALU.add, RB(j), [Brt[j]])
        for j in J3:
            recip(rc(j, 3), rc(j, 2), RB(j), [Brt[j]])
        for j in J3:
            P.op("dve", lambda e, j=j: e.max(out=top8[:, j, :], in_=lem[:, j, :]), [Brt[j]], [Brt[j]])
        for j in J3:
            ts("dve", m1[:, j, :], lem[:, j, :], top8[:, j, 0:1], ALU.is_equal, [Brt[j]], [Brt[j]])
        for j in J3:
            ts("dve", m2[:, j, :], lem[:, j, :], top8[:, j, 1:2], ALU.is_equal, [Brt[j]], [Brt[j]])
        for j in J3:
            tt("dve", rc(j, 4), top8[:, j, 1:2], top8[:, j, 0:1], ALU.subtract, [Brt[j]], [Brt[j]])
        for j in J3:
            act(rc(j, 5), rc(j, 4), AF.Exp, [Brt[j]], [Brt[j]])
        for j in J3:
            ts("dve", rc(j, 6), rc(j, 5), 1.0, ALU.add, [Brt[j]], [Brt[j]])
        for j in J3:
            recip(rc(j, 6), rc(j, 6), [Brt[j]], [Brt[j]])
        for j in J3:
            tt("dve", rc(j, 7), rc(j, 5), rc(j, 6), ALU.mult, [Brt[j]], [Brt[j]])
        for j in J3:
            tt("dve", rc(j, 8), rc(j, 6), rc(j, 3), ALU.mult, [Brt[j]], [Brt[j]])
        for j in J3:
            tt("dve", rc(j, 9), rc(j, 7), rc(j, 3), ALU.mult, [Brt[j]], [Brt[j]])
        for j in J3:
            ts("dve", m1[:, j, :], m1[:, j, :], rc(j, 8), ALU.mult, [Brt[j]], [Brt[j]])
        for j in J3:
            stt(gates[:, j, :], m2[:, j, :], rc(j, 9), m1[:, j, :], ALU.mult, ALU.add, [Brt[j]], [Bgates[j]])
        if stop == '2a':
            continue

        Wm = min(W, L - tok0)
        slot = [0]

        def load_expert(e_):
            idx = []
            for src in (w1_bf[e_].rearrange("(k p) c -> p k c", p=128), w3_bf[e_].rearrange("(k p) c -> p k c", p=128)):
                i = slot[0] % 7
                slot[0] += 1
                P.dma("sp", lambda e, i=i, src=src: e.dma_start(out=wslot[i][:], in_=src), reads=[Bexp[e_]], writes=[Bws[i]])
                idx.append(i)
            i = slot[0] % 7
            slot[0] += 1
            dst = wslot[i][:, :, :].rearrange("p a b -> p (a b)").rearrange("p (c n) -> p c n", c=4)
            src = w2_bf[e_].rearrange("(c p) n -> p c n", p=128)
            P.dma("sp", lambda e, dst=dst, src=src: e.dma_start(out=dst, in_=src), reads=[Bexp[e_]], writes=[Bws[i]])
            idx.append(i)
            return idx

        def gate_bcast(e_):
            gi = e_ % 2
            for j in range(SBT):
                di = j % 2
                ts("dve", De[di][:], ident_b[:], gates[:, j, e_:e_ + 1], ALU.mult, [Bconst, Bgates[j]], [BDe[di]])
                mm(psf[3][:, j * 128:(j + 1) * 128], ones_b[:], De[di][:], j == 0, j == SBT - 1, [Bconst, BDe[di]], [PB[3]])
            act(Gb[gi][:], psf[3][:, 0:W], AF.Copy, [PB[3]], [BGb[gi]])

        pbank = [0]

        def hidden_chunk(e_, c, i1, i3, part="AB"):
            gi = e_ % 2
            hi_ = e_ % 2
            if part == "B":
                bi = c % 2
                tt(PL, hidg[hi_][:, c, 0:Wm], u_sb[bi][:, 0:Wm], Gb[gi][:, 0:Wm], ALU.mult, [Bu[bi], BGb[gi]], [Bhidg[hi_]])
                return
            bi = pbank[0] % 2
            pbank[0] += 1
            b1, b3 = (4, 5) if bi == 0 else (6, 7)
            for k in range(8):
                mm(psf[b1][:, 0:Wm], wslot[i1][:, k, c * 128:(c + 1) * 128], h1T_b[:, k, 0:Wm], k == 0, k == 7,
                   [Bws[i1]] + Bh1Tb, [PB[b1]])
            for k in range(8):
                mm(psf[b3][:, 0:Wm], wslot[i3][:, k, c * 128:(c + 1) * 128], h1T_b[:, k, 0:Wm], k == 0, k == 7,
                   [Bws[i3]] + Bh1Tb, [PB[b3]])
            act(s_sb[bi][:, 0:Wm], psf[b1][:, 0:Wm], AF.Silu, [PB[b1]], [Bs[bi]])
            tt("dve", u_sb[bi][:, 0:Wm], psf[b3][:, 0:Wm], s_sb[bi][:, 0:Wm], ALU.mult, [PB[b3], Bs[bi]], [Bu[bi]])
            if part == "A":
                return
            tt(PL, hidg[hi_][:, c, 0:Wm], u_sb[bi][:, 0:Wm], Gb[gi][:, 0:Wm], ALU.mult, [Bu[bi], BGb[gi]], [Bhidg[hi_]])

        def down_proj(e_, i2):
            hi_ = e_ % 2
            w2v = wslot[i2][:, :, :].rearrange("p a b -> p (a b)")
            for j in range(SBT):
                for half in range(2):
                    b = next_bank(0, 3)
                    for c in range(4):
                        mm(psf[b][:, :], hidg[hi_][:, c, j * 128:(j + 1) * 128],
                           w2v[:, c * 1024 + half * 512:c * 1024 + (half + 1) * 512], c == 0, c == 3,
                           [Bhidg[hi_], Bws[i2]], [PB[b]])
                    hs = slice(half * 512, (half + 1) * 512)
                    tt("dve", h1[:, j, hs], psf[b][:, :], h1[:, j, hs], ALU.add, [PB[b], Bh1[j]], [Bh1[j]])

        widx = {0: load_expert(0)}
        pbank[0] = 0
        for e_ in range(NE):
            i1, i3, i2 = widx[e_]
            hidden_chunk(e_, 0, i1, i3, "A" if e_ == 0 else "AB")
            if e_ + 1 < NE:
                widx[e_ + 1] = load_expert(e_ + 1)
            if e_ > 0:
                down_proj(e_ - 1, widx[e_ - 1][2])
            if e_ == 0:
                hidden_chunk(e_, 1, i1, i3, "A")
                gate_bcast(0)
                hidden_chunk(e_, 0, i1, i3, "B")
                hidden_chunk(e_, 1, i1, i3, "B")
            if e_ + 1 < NE:
                gate_bcast(e_ + 1)
            for c in range(2 if e_ == 0 else 1, 4):
                hidden_chunk(e_, c, i1, i3)
        down_proj(NE - 1, widx[NE - 1][2])
        if stop == '2b':
            continue
        P.barrier()
        ln2_def = []
        P.defer = ln2_def
        nr = [TT[:, 2 * j:2 * j + 2, :].rearrange("p a b -> p (a b)") for j in range(SBT)]
        Bnr = [BT[2 * j:2 * j + 2] for j in range(SBT)]
        layer_norm3([h1[:, j, :] for j in range(SBT)], [[Bh1[j]] for j in range(SBT)], nr, Bnr, ln2g, ln2b, nr, Bnr)
        for j in range(SBT):
            i = t0 + j
            ot = nr[j]
            if i == 0:
                P.dma("sp", lambda e, ot=ot: e.dma_start(out=out_d[0:112, :], in_=ot[16:128, :]), reads=Bnr[j])
            elif i < NT - 1:
                P.dma("sp", lambda e, ot=ot, i=i: e.dma_start(out=out_d[128 * i - 16:128 * i + 112, :], in_=ot[:, :]), reads=Bnr[j])
            else:
                P.dma("sp", lambda e, ot=ot: e.dma_start(out=out_d[SEQ - 16:SEQ, :], in_=ot[0:16, :]), reads=Bnr[j])
        P.defer = None
        pending_ln2[0] = ln2_def
        if Q == min(NSB, maxq) - 1:
            flush_ln2(len(ln2_def))

    P.emit()
    return nc


_CACHE = {}


def _prep_inputs(inputs, b):
    f = lambda a: np.ascontiguousarray(np.asarray(a, dtype=np.float32))
    m = {
        "x": f(inputs["x"][b]),
        "meta_tokens": f(inputs["meta_tokens"]),
        "w_in": f(inputs["w_in"][0]),
        "hg_lower_bound": f(inputs["hg_lower_bound"]),
        "hg_norm_g": f(inputs["hg_norm_g"]),
        "sb_norm_g": f(inputs["sb_norm_g"]),
        "w_out": f(inputs["w_out"][0]),
        "ln1_g": f(inputs["ln1_g"]), "ln1_b": f(inputs["ln1_b"]),
        "w_router_group": f(inputs["w_router_group"][0]),
        "b_router_group": f(inputs["b_router_group"]),
        "w_router_expert": f(np.asarray(inputs["w_router_expert"][0]).reshape(D, 16)),
        "b_router_expert": f(np.asarray(inputs["b_router_expert"]).reshape(1, 16)),
        "w_exp_gate": f(inputs["w_exp_gate"][0]),
        "w_exp_up": f(inputs["w_exp_up"][0]),
        "w_exp_down": f(inputs["w_exp_down"][0]),
        "ln2_g": f(inputs["ln2_g"]), "ln2_b": f(inputs["ln2_b"]),
    }
    m.update(host_consts())
    return m


def kernel(**inputs):
    nc = bass.Bass("TRN2", target_bir_lowering=False)
    build(nc)
    n = 8
    in_maps = [_prep_inputs(inputs, b) for b in range(n)]
    res = run_bass_kernel_spmd(nc, in_maps, core_ids=list(range(n)))
    return np.stack([np.asarray(r["out"], dtype=np.float32) for r in res.results], axis=0)
```
